# Optimizing a Trainium2 kernel written in Bass

```python
import math
import jax, jax.numpy as jnp
from jax import lax
import numpy as np

D_MODEL = 1024
BATCH = 8
SEQ = 2048
DEPTH = 2

N_HEADS = 4
HEAD_DIM = 64
V_DIM = 2 * HEAD_DIM
ATTN_QK = N_HEADS * 2 * HEAD_DIM
ATTN_V = N_HEADS * V_DIM
ROPE_THETA = 10000.0
Q_BLOCK = 128
CONV_CH = 512
CONV_K = 31
CONV_PAD = (CONV_K - 1) // 2
IN_SPLITS = (ATTN_QK, 2 * ATTN_QK, 2 * ATTN_QK + ATTN_V,
             2 * ATTN_QK + ATTN_V + 2 * CONV_CH)
IN_COLS = 2 * ATTN_QK + ATTN_V + 2 * CONV_CH + 2 * D_MODEL
D_FF = 2816
N_EXPERTS = 8
TOP_K = 2
D_EXPERT = 3584
N_DENSE = (DEPTH + 1) // 2
N_MOE = DEPTH // 2
PLE_DIM = 256
EPS = 1e-6

kernel_name = "hybrid_diffattn_conformer_moe_encoder"


def rms_norm(x, g):
    xf = x.astype(jnp.float32)
    y = xf * lax.rsqrt(jnp.mean(xf * xf, axis=-1, keepdims=True) + EPS)
    return (y * g.astype(jnp.float32)).astype(x.dtype)


def layer_norm(x, g, b):
    xf = x.astype(jnp.float32)
    mu = jnp.mean(xf, axis=-1, keepdims=True)
    xc = xf - mu
    y = xc * lax.rsqrt(jnp.mean(xc * xc, axis=-1, keepdims=True) + EPS)
    return (y * g.astype(jnp.float32) + b.astype(jnp.float32)).astype(x.dtype)


def rope_tables(seq, dtype):
    inv_freq = ROPE_THETA ** (-jnp.arange(0, HEAD_DIM, 2, dtype=jnp.float32) / HEAD_DIM)
    ang = jnp.arange(seq, dtype=jnp.float32)[:, None] * inv_freq[None, :]
    return jnp.cos(ang).astype(dtype), jnp.sin(ang).astype(dtype)


def apply_rope(t, cos, sin):
    t1, t2 = jnp.split(t, 2, axis=-1)
    c = cos[:, None, None, :]
    s = sin[:, None, None, :]
    return jnp.concatenate([t1 * c - t2 * s, t2 * c + t1 * s], axis=-1)


def diff_attention(q, k, v, lam):
    b, s = q.shape[0], q.shape[1]
    nb = s // Q_BLOCK
    qb = q.reshape(b, nb, Q_BLOCK, N_HEADS, 2, HEAD_DIM).transpose(1, 0, 2, 3, 4, 5)
    scale = HEAD_DIM ** -0.5

    def one_block(qi):
        sc = jnp.einsum('bqhcd,bkhcd->bhcqk', qi, k).astype(jnp.float32) * scale
        pr = jax.nn.softmax(sc, axis=-1)
        a = pr[:, :, 0] - lam * pr[:, :, 1]
        return jnp.einsum('bhqk,bkhe->bqhe', a.astype(v.dtype), v)

    o = lax.map(one_block, qb)
    return o.transpose(1, 0, 2, 3, 4).reshape(b, s, N_HEADS, V_DIM)


def conformer_conv(u, conv_w, conv_b, ln_g, ln_b, w_out):
    a, g = jnp.split(u, 2, axis=-1)
    z = a * jax.nn.sigmoid(g)
    z = lax.conv_general_dilated(
        z, conv_w[:, None, :], window_strides=(1,), padding=[(CONV_PAD, CONV_PAD)],
        dimension_numbers=('NWC', 'WIO', 'NWC'), feature_group_count=CONV_CH) + conv_b
    z = jax.nn.silu(layer_norm(z, ln_g, ln_b))
    return z @ w_out


def swiglu(h, w_gu, w_down):
    g, u = jnp.split(h @ w_gu, 2, axis=-1)
    return (jax.nn.silu(g) * u) @ w_down


def moe_swiglu(h, w_router, b_router, we_gu, we_down):
    b, s, d = h.shape
    t = h.reshape(b * s, d)
    logits = (t @ w_router).astype(jnp.float32) + b_router.astype(jnp.float32)
    top_v, top_i = lax.top_k(logits, TOP_K)
    top_w = jax.nn.softmax(top_v, axis=-1)
    combine = jnp.sum(jax.nn.one_hot(top_i, N_EXPERTS, dtype=jnp.float32) * top_w[..., None], axis=1)
    combine = combine.astype(h.dtype)
    out = jnp.zeros_like(t)
    for e in range(N_EXPERTS):
        out = out + combine[:, e:e + 1] * swiglu(t, we_gu[e], we_down[e])
    return out.reshape(b, s, d)


def setup_inputs(seed: int = 0) -> dict:
    key = jax.random.key(seed)
    ks = iter(jax.random.split(key, 40))

    def w(shape, fan_in):
        return jax.random.normal(next(ks), shape, jnp.float32) * (fan_in ** -0.5)

    def gain(shape):
        return 1.0 + 0.02 * jax.random.normal(next(ks), shape, jnp.float32)

    def bias(shape, scale=0.02):
        return scale * jax.random.normal(next(ks), shape, jnp.float32)

    return {
        "x": jax.random.normal(next(ks), (BATCH, SEQ, D_MODEL), jnp.float32),
        "p": jax.random.normal(next(ks), (DEPTH, BATCH, SEQ, PLE_DIM), jnp.float32),
        "g_mix": gain((DEPTH, D_MODEL)),
        "w_in": w((DEPTH, D_MODEL, IN_COLS), D_MODEL),
        "lam": 0.1 * jax.random.normal(next(ks), (DEPTH, 4, HEAD_DIM), jnp.float32),
        "g_subln": gain((DEPTH, V_DIM)),
        "w_attn_out": w((DEPTH, ATTN_V, D_MODEL), ATTN_V),
        "conv_w": w((DEPTH, CONV_K, CONV_CH), CONV_K),
        "conv_b": bias((DEPTH, CONV_CH)),
        "conv_ln_g": gain((DEPTH, CONV_CH)),
        "conv_ln_b": bias((DEPTH, CONV_CH)),
        "w_conv_out": w((DEPTH, CONV_CH, D_MODEL), CONV_CH),
        "w_o": w((DEPTH, D_MODEL, D_MODEL), D_MODEL),
        "g_ffn": gain((DEPTH, D_MODEL)),
        "w_ff_gu": w((N_DENSE, D_MODEL, 2 * D_FF), D_MODEL),
        "w_ff_down": w((N_DENSE, D_FF, D_MODEL), D_FF),
        "w_router": w((N_MOE, D_MODEL, N_EXPERTS), D_MODEL),
        "b_router": bias((N_MOE, N_EXPERTS), 0.01),
        "we_gu": w((N_MOE, N_EXPERTS, D_MODEL, 2 * D_EXPERT), D_MODEL),
        "we_down": w((N_MOE, N_EXPERTS, D_EXPERT, D_MODEL), D_EXPERT),
        "g_ple": gain((DEPTH, D_MODEL)),
        "w_ple_gate": w((DEPTH, D_MODEL, D_MODEL), D_MODEL),
        "w_ple_proj": w((DEPTH, PLE_DIM, D_MODEL), PLE_DIM),
        "g_final": gain((D_MODEL,)),
    }


def reference(x, p, g_mix, w_in, lam, g_subln, w_attn_out, conv_w, conv_b, conv_ln_g,
              conv_ln_b, w_conv_out, w_o, g_ffn, w_ff_gu, w_ff_down, w_router, b_router,
              we_gu, we_down, g_ple, w_ple_gate, w_ple_proj, g_final):
    b, s, _ = x.shape
    cos, sin = rope_tables(s, x.dtype)
    for i in range(DEPTH):
        h = rms_norm(x, g_mix[i])
        q, k, v, u, gl = jnp.split(h @ w_in[i], IN_SPLITS, axis=-1)
        q = apply_rope(q.reshape(b, s, N_HEADS, 2, HEAD_DIM), cos, sin)
        k = apply_rope(k.reshape(b, s, N_HEADS, 2, HEAD_DIM), cos, sin)
        v = v.reshape(b, s, N_HEADS, V_DIM)
        lam_init = 0.8 - 0.6 * math.exp(-0.3 * i)
        lf = lam[i].astype(jnp.float32)
        lam_full = (jnp.exp(jnp.sum(lf[0] * lf[1])) - jnp.exp(jnp.sum(lf[2] * lf[3]))
                    + lam_init)
        o = diff_attention(q, k, v, lam_full)
        o = rms_norm(o, g_subln[i]) * (1.0 - lam_init)
        attn_d = o.reshape(b, s, ATTN_V) @ w_attn_out[i]
        conv_d = conformer_conv(u, conv_w[i], conv_b[i], conv_ln_g[i], conv_ln_b[i],
                                w_conv_out[i])
        ga, gc = jnp.split(gl, 2, axis=-1)
        x = x + (jax.nn.sigmoid(ga) * attn_d + jax.nn.sigmoid(gc) * conv_d) @ w_o[i]
        h = rms_norm(x, g_ffn[i])
        if i % 2 == 0:
            x = x + swiglu(h, w_ff_gu[i // 2], w_ff_down[i // 2])
        else:
            m = i // 2
            x = x + moe_swiglu(h, w_router[m], b_router[m], we_gu[m], we_down[m])
        h = rms_norm(x, g_ple[i])
        x = x + jax.nn.sigmoid(h @ w_ple_gate[i]) * (p[i] @ w_ple_proj[i])
    return rms_norm(x, g_final)
```

```python
import math
import os
import numpy as np
from contextlib import ExitStack
import concourse.bass as bass
import concourse.mybir as mybir
from concourse.bass_utils import run_bass_kernel_spmd

F32 = mybir.dt.float32
BF16 = mybir.dt.bfloat16
AF = mybir.ActivationFunctionType
ALU = mybir.AluOpType

D = 1024; S = 2048; DEPTH = 2; NH = 4; HD = 64; VD = 128; CCH = 512; CK = 31
DFF = 2816; NE = 8; DEX = 3584; PLE = 256; INC = 4608
EPS = 1e-6
NTC = 4; TCW = 512
PL = 417
O_GMIX, O_GFFN, O_GPLE, O_GSUB, O_CB, O_LNG, O_LNB, O_CW, O_LAM = 0, 8, 16, 24, 25, 29, 33, 37, 161
O_GFIN = 2 * PL; O_BR = O_GFIN + 8; O_WR = O_BR + 8; O_EPS = O_WR + 64; NP = O_EPS + 1
NS_, NB_, LA_ = 2, 3, 2
HOIST = os.environ.get("KNOHOIST") is None


class Buf:
    __slots__ = ("name", "w", "r", "sem", "cnt")
    REG = []

    def __init__(self, name=""):
        self.name = name; self.w = None; self.r = {}; self.sem = None; self.cnt = 0
        Buf.REG.append(self)


class Tracker:
    def __init__(self, nc, es):
        self.nc = nc; self.es = es
        self.engs = ["pe", "act", "dve", "pool", "sp"]
        self.semh = {}
        for e in ["pe", "act", "dve", "pool"]:
            self.semh[e] = es.enter_context(nc.semaphore("s_" + e))
        self.cnt = {e: 0 for e in ["pe", "act", "dve", "pool"]}
        self.seen = {e: {} for e in self.engs}
        self.prog = {e: [] for e in self.engs}
        self.dbufs = []
        self.regs = {}

    def load_flag(self, flag_ap, flagb, n):
        for e in self.engs:
            w = self._waits(e, [flagb], [])
            for i in range(n):
                def f(h, e=e, i=i):
                    r = h.alloc_register("flag_%s_%d" % (e, i))
                    ins = h.reg_load(r, flag_ap[0:1, i:i + 1])
                    self.regs[(e, i)] = h.snap(r)
                    return ins
                self.prog[e].append((w if i == 0 else [], f, None, 0))

    def skip_begin(self, fidx):
        if not hasattr(self, "_sstack"):
            self._sstack = []
        self._sstack.append((dict(self.cnt), {e: dict(d) for e, d in self.seen.items()}))
        for e in ("pe", "act", "dve"):
            self.prog[e].append(("IFR", fidx, None, 0))

    def skip_end(self):
        s_cnt, s_seen = self._sstack.pop()
        for e in ("pe", "act", "dve"):
            d = self.cnt[e] - s_cnt[e]
            self.prog[e].append(("ELSE", None, None, 0))
            if d > 0:
                self.prog[e].append(([], "INC", e, d))
            self.prog[e].append(("END", None, None, 0))
        assert self.cnt["pool"] == s_cnt["pool"]
        self.seen = s_seen

    def _dsem(self, buf):
        if buf.sem is None:
            key = "d%d" % len(self.dbufs)
            buf.sem = key
            self.dbufs.append(buf)
            self.semh[key] = self.es.enter_context(self.nc.semaphore(key))
        return buf.sem

    def _waits(self, eng, reads, writes):
        ev = {}
        for b in reads:
            if b.w is not None and ev.get(b.w[0], 0) < b.w[1]:
                ev[b.w[0]] = b.w[1]
        for b in writes:
            if b.w is not None and ev.get(b.w[0], 0) < b.w[1]:
                ev[b.w[0]] = b.w[1]
            for k, v in b.r.items():
                if ev.get(k, 0) < v:
                    ev[k] = v
        out = []
        for k, v in ev.items():
            if eng == "pe" and k == "pe":
                continue
            if self.seen[eng].get(k, 0) >= v:
                continue
            self.seen[eng][k] = v
            out.append((k, v))
        return out

    def op(self, eng, fn, reads=(), writes=(), inc=True):
        w = self._waits(eng, reads, writes)
        if inc:
            self.cnt[eng] += 1
            ev = (eng, self.cnt[eng])
        else:
            ev = (eng, self.cnt[eng] + 1)
        self.prog[eng].append((w, fn, eng if inc else None, 1))
        for b in reads:
            if b.r.get(ev[0], 0) < ev[1]:
                b.r[ev[0]] = ev[1]
        for b in writes:
            b.w = ev; b.r = {}

    def dma(self, fns, dst, reads=(), q="sp", also=()):
        w = self._waits(q, reads, [dst] + list(also))
        key = self._dsem(dst)
        for i, fn in enumerate(fns):
            self.prog[q].append((w if i == 0 else [], fn, key, 16))
        dst.cnt += 16 * len(fns)
        ev = (key, dst.cnt)
        for b in reads:
            if b.r.get(key, 0) < ev[1]:
                b.r[key] = ev[1]
        dst.w = ev; dst.r = {}
        for b in also:
            b.w = ev; b.r = {}
        return ev

    def barrier(self):
        tg = [(e, self.cnt[e]) for e in ["pe", "act", "dve", "pool"] if self.cnt[e] > 0]
        tg += [(b.sem, b.cnt) for b in self.dbufs]
        for e in self.engs:
            w = []
            for k, v in tg:
                if e == "pe" and k == "pe":
                    continue
                if self.seen[e].get(k, 0) >= v:
                    continue
                self.seen[e][k] = v
                w.append((k, v))
            if w:
                self.prog[e].append((w, None, None, 0))

    def _snap(self):
        return (dict(self.cnt), {e: dict(d) for e, d in self.seen.items()},
                [(b, b.w, dict(b.r), b.cnt) for b in Buf.REG])

    def _restore(self, st):
        self.cnt = dict(st[0]); self.seen = {e: dict(d) for e, d in st[1].items()}
        for (b, w, r, c) in st[2]:
            b.w = w; b.r = dict(r); b.cnt = c

    def _endstate(self):
        d = dict(self.cnt)
        for b in self.dbufs:
            d[b.sem] = b.cnt
        return d

    def cond_begin(self):
        self._c0 = self._snap()
        self._ndb0 = len(self.dbufs)
        for e in self.engs:
            self.prog[e].append(("IF", None, None, 0))

    def cond_else(self):
        self.barrier()
        self._e1 = self._endstate()
        self._else_idx = {e: len(self.prog[e]) for e in self.engs}
        for e in self.engs:
            self.prog[e].append(("ELSE", None, None, 0))
        self._restore(self._c0)

    def cond_end(self):
        self.barrier()
        e2 = self._endstate(); e1 = self._e1
        keys = set(e1) | set(e2)
        own = {"pe": "pe", "act": "act", "dve": "dve", "pool": "pool"}
        for k in keys:
            a = e1.get(k, 0); b_ = e2.get(k, 0); tgt = max(a, b_)
            eng = own.get(k, "sp")
            if tgt > a:
                self.prog[eng].insert(self._else_idx[eng], ([], "INC", k, tgt - a))
                for e in self.engs:
                    if e != eng:
                        pass
                self._else_idx[eng] += 1
            if tgt > b_:
                self.prog[eng].append(([], "INC", k, tgt - b_))
            if k in self.cnt:
                self.cnt[k] = tgt
        for b in self.dbufs:
            b.cnt = max(e1.get(b.sem, 0), e2.get(b.sem, 0))
        for e in self.engs:
            self.prog[e].append(("END", None, None, 0))
            for k in keys:
                self.seen[e][k] = max(e1.get(k, 0), e2.get(k, 0))
        for b in Buf.REG:
            b.w = None; b.r = {}

    def simulate(self, take_if=True, take_ifr=True):
        sem = {}
        ptr = {e: 0 for e in self.engs}
        mode = {e: 0 for e in self.engs}
        inner = {e: [] for e in self.engs}
        total = sum(len(p) for p in self.prog.values())
        done = 0
        while True:
            prog_made = False
            for e in self.engs:
                p = self.prog[e]
                while ptr[e] < len(p):
                    w, fn, sk, amt = p[ptr[e]]
                    if w == "IFR" or (inner[e] and w in ("ELSE", "END")):
                        if w == "IFR":
                            inner[e].append(1)
                        elif w == "ELSE":
                            inner[e][-1] = 2
                        else:
                            inner[e].pop()
                        ptr[e] += 1; done += 1; prog_made = True
                        continue
                    if w in ("IF", "ELSE", "END"):
                        mode[e] = {"IF": 1, "ELSE": 2, "END": 0}[w]
                        ptr[e] += 1; done += 1; prog_made = True
                        continue
                    if (mode[e] == 1 and not take_if) or (mode[e] == 2 and take_if) or \
                            any((m_ == 1 and not take_ifr) or (m_ == 2 and take_ifr) for m_ in inner[e]):
                        ptr[e] += 1; done += 1; prog_made = True
                        continue
                    if any(sem.get(k, 0) < v for k, v in w):
                        break
                    if sk is not None:
                        sem[sk] = sem.get(sk, 0) + amt
                    ptr[e] += 1; done += 1; prog_made = True
            if done == total:
                return True
            if not prog_made:
                for e in self.engs:
                    if ptr[e] < len(self.prog[e]):
                        w = self.prog[e][ptr[e]][0]
                        print("DEADLOCK", e, ptr[e], len(self.prog[e]), [(k, v, sem.get(k, 0)) for k, v in w if sem.get(k, 0) < v])
                return False

    def emit(self):
        nc = self.nc
        hmap = {"pe": "tensor", "act": "scalar", "dve": "vector", "pool": "gpsimd", "sp": "sync"}
        semh = self.semh
        with nc.Block() as block:
            for e, attr in hmap.items():
                prog = self.prog[e]

                def body(h, prog=prog, e=e):
                    stack = []
                    n = len(prog); idx = 0
                    while idx < n:
                        w, fn, sk, amt = prog[idx]
                        if w == "IF":
                            cm = h.If(self.regs[(e, 0)] > 0); cm.__enter__(); stack.append([cm, None])
                            idx += 1; continue
                        if w == "IFR":
                            cm = h.If(self.regs[(e, fn)] > 0); cm.__enter__(); stack.append([cm, None])
                            idx += 1; continue
                        if w == "ELSE":
                            ent = stack.pop(); ent[0].__exit__(None, None, None)
                            cm = h.Else(); cm.__enter__(); stack.append([cm, None])
                            idx += 1
                            if ent[1] is not None:
                                hfn, hsk, hamt = ent[1]
                                d = 0
                                if idx < n and prog[idx][1] == "INC" and prog[idx][2] == e:
                                    d = prog[idx][3]; idx += 1
                                ins = hfn(h)
                                tot = (hamt if hsk is not None else 0) + d
                                if tot > 0:
                                    ins.then_inc(semh[e], tot)
                            continue
                        if w == "END":
                            stack.pop()[0].__exit__(None, None, None)
                            idx += 1; continue
                        for (k, v) in w:
                            h.wait_ge(semh[k], v)
                        if fn is None:
                            idx += 1; continue
                        if fn == "INC":
                            if sk == e:
                                h.drain().then_inc(semh[sk], amt)
                            else:
                                h.sem_inc(semh[sk], amt)
                            idx += 1; continue
                        nxt = prog[idx + 1] if idx + 1 < n else None
                        if HOIST and nxt is not None and nxt[0] == "IFR" and (sk is None or sk == e):
                            cm = h.If(self.regs[(e, nxt[1])] > 0); cm.__enter__(); stack.append([cm, (fn, sk, amt)])
                            ins = fn(h)
                            if sk is not None:
                                ins.then_inc(semh[sk], amt)
                            idx += 2; continue
                        ins = fn(h)
                        if sk is not None:
                            ins.then_inc(semh[sk], amt)
                        idx += 1
                getattr(block, attr)(body)


def build(plan, debug_stop=None):
    nc = bass.Bass("TRN2", target_bir_lowering=False)
    es = ExitStack()
    Buf.REG = []
    T = Tracker(nc, es)

    def din(name, shape):
        return nc.dram_tensor(name, shape, F32, kind="ExternalInput").ap()

    xT_d = din("xT", [D, S]); pT_d = din("pT", [DEPTH, PLE, S])
    WD = {
        "w_in": din("w_in", [DEPTH, D, INC]), "w_qkp": din("w_qkp", [DEPTH, D, 1024]),
        "w_ao": din("w_attn_out", [DEPTH, 512, D]), "w_co": din("w_conv_out", [DEPTH, 512, D]),
        "w_o": din("w_o", [DEPTH, D, D]), "w_gu": din("w_ff_gu", [1, D, 2 * DFF]),
        "w_dn": din("w_ff_down", [1, DFF, D]), "we_gu": din("we_gu", [NE, D, 2 * DEX]),
        "we_dn": din("we_down", [NE, DEX, D]), "w_pg": din("w_ple_gate", [DEPTH, D, D]),
        "w_pp": din("w_ple_proj", [DEPTH, PLE, D]),
    }
    prm_d = din("prm", [128, NP]); rope_d = din("rope", [2, 128, S]); ident_d = din("ident", [128, 128])
    out_d = nc.dram_tensor("outT", [D, S], F32, kind="ExternalOutput").ap()
    cst_d = din("cst", [128, 1184])
    flag_d = nc.dram_tensor("flag_scr", [1, 32], mybir.dt.int32, kind="Internal").ap()

    def sb(name, shape, dt):
        return es.enter_context(nc.sbuf_tensor("sb_" + name, shape, dt))

    x = sb("x", [128, 8, S], F32); xb = [Buf("x%d" % i) for i in range(NTC)]
    wst = [sb("wst%d" % i, [128, 2048], F32) for i in range(NS_)]
    wbf = [sb("wbf%d" % i, [128, 2048], BF16) for i in range(NB_)]
    rstd = sb("rstd", [128, S], F32); rstdb = [Buf("rs%d" % i) for i in range(NTC)]
    prm = sb("prm", [128, NP], F32); prmb = Buf("prm")
    ropeT = sb("ropeT", [128, 2, S], BF16); ropeb = Buf("rope")
    ones_bf = sb("ones_bf", [128, 128], BF16); ones_f = sb("ones_f", [128, 128], F32)
    ident = sb("ident", [128, 128], F32); constb = Buf("const")
    scr = sb("scr", [128, 16], F32); scrb = Buf("scr")
    comb = sb("comb", [128, 16, 8], F32); combb = Buf("comb")
    rt = sb("rt", [128, 64], F32); rtb = Buf("rt")
    ARENA_B = 93440
    arena = sb("arena", [128, ARENA_B // 2], BF16)

    def view(off, dt, shape):
        n = 1
        for s_ in shape:
            n *= s_
        esz = 4 if dt == F32 else 2
        a = arena[:, off // 2: off // 2 + n * esz // 2]
        if dt == F32:
            a = a.bitcast(F32)
        if len(shape) == 2:
            a = a.rearrange("p (a b) -> p a b", a=shape[0])
        return a

    psum = [es.enter_context(nc.psum_tensor("ps%d" % i, [128, 512], F32))[:, :] for i in range(8)]
    psb = [Buf("ps%d" % i) for i in range(8)]
    psi = [0]

    def next_ps():
        i = psi[0] % 8; psi[0] += 1
        return psum[i], psb[i]

    R0, R1, R2, RT = 0, 32768, 65536, 82176
    tmpf = [view(RT + i * 2048, F32, [512]) for i in range(4)]; tmpfb = [Buf("tf%d" % i) for i in range(4)]
    sqt = [view(RT + 8192 + i * 1024, BF16, [512]) for i in range(3)]; sqtb = [Buf("sq%d" % i) for i in range(3)]
    sqi = [0]

    def next_sq():
        i = sqi[0] % 3; sqi[0] += 1
        return sqt[i], sqtb[i]

    CAP = 1024
    USE_SKIP = os.environ.get("KNOSKIP") is None
    maskf = view(86016, F32, [16, 8]); posf = view(86528, F32, [16, 8])
    tokw = view(87040, F32, [3, 8]); sinfo = view(87168, F32, [8, 4])
    maskb = view(87296, BF16, [16, 8]); mkb = Buf("mask")
    zlhs = view(91648, BF16, [128])
    hscr_d = nc.dram_tensor("h_scr", [128, 16 * 1024], BF16, kind="Internal").ap()
    flagi = sb("flagi", [128, 20], mybir.dt.int32); flagib = Buf("flagi")

    def ACT(out, in_, func, reads, writes, scale=None, bias=None):
        kw = {}
        if scale is not None:
            kw["scale"] = scale
        if bias is not None:
            kw["bias"] = bias
        T.op("act", lambda h: h.activation(out=out, in_=in_, func=func, **kw), reads, writes)

    def ACOPY(out, in_, reads, writes):
        T.op("act", lambda h: h.copy(out=out, in_=in_), reads, writes)

    def TT(eng, out, a, b, op, reads, writes):
        T.op(eng, lambda h: h.tensor_tensor(out=out, in0=a, in1=b, op=op), reads, writes)

    def TS(eng, out, a, s1, s2, op0, op1, reads, writes):
        if op1 is None:
            T.op(eng, lambda h: h.tensor_scalar(out=out, in0=a, scalar1=s1, scalar2=None, op0=op0), reads, writes)
        else:
            T.op(eng, lambda h: h.tensor_scalar(out=out, in0=a, scalar1=s1, scalar2=s2, op0=op0, op1=op1), reads, writes)

    def STT(out, a, s, b, op0, op1, reads, writes):
        T.op("dve", lambda h: h.scalar_tensor_tensor(out=out, in0=a, scalar=s, in1=b, op0=op0, op1=op1), reads, writes)

    def RECIP(out, in_, reads, writes):
        T.op("dve", lambda h: h.reciprocal(out=out, in_=in_), reads, writes)

    def MM(ps, lhsT, rhs, start, stop, reads, writes, inc=False):
        T.op("pe", lambda h: h.matmul(ps, lhsT=lhsT, rhs=rhs, start=start, stop=stop), reads, writes,
             inc=(stop or inc))

    def DMA(out, in_, dst, reads=()):
        T.dma([lambda h: h.dma_start(out=out, in_=in_)], dst, reads)

    class WS:
        def __init__(self):
            self.stb = [Buf("st%d" % i) for i in range(NS_)]
            self.bfb = [Buf("bf%d" % i) for i in range(NB_)]
            self.rec = []; self.i = 0; self.issued = 0; self.fences = []; self.pending = []

        def fence(self):
            self.rec.append("FENCE")
            self.i += 1
            if plan is not None:
                self.issued = max(self.issued, self.i)

        def _src(self, seg):
            name, idx, k0, nk, c0, ncols = seg
            W = WD[name][idx]
            return W[k0 * 128:(k0 + nk) * 128, c0:c0 + ncols].rearrange("(k p) c -> p k c", p=128)

        def _issue(self, t, now=False):
            tile = plan[t]; s = t % NS_; b = t % NB_; off = 0; fns = []
            for seg in tile:
                nk, ncols = seg[3], seg[5]
                dst = wst[s][:, off:off + nk * ncols].rearrange("p (k c) -> p k c", k=nk)
                src = self._src(seg)
                fns.append(lambda h, dst=dst, src=src: h.dma_start(out=dst, in_=src))
                off += nk * ncols
            T.dma(fns, self.stb[s])
            o_ = wbf[b][:, 0:off]; i_ = wst[s][:, 0:off]
            eng = ("act", "dve")[t % 2]

            def cast(eng=eng, o_=o_, i_=i_, s=s, b=b):
                if eng == "act":
                    T.op("act", lambda h: h.copy(out=o_, in_=i_), [self.stb[s]], [self.bfb[b]])
                else:
                    T.op(eng, lambda h: h.tensor_copy(out=o_, in_=i_), [self.stb[s]], [self.bfb[b]])
            if eng == "pool" or now:
                cast()
            else:
                self.pending.append(cast)

        def next(self, tile):
            t = self.i; self.i += 1
            self.rec.append(tile)
            if plan is not None:
                assert plan[t] == tile, (t, plan[t], tile)
                for c_ in self.pending:
                    c_()
                self.pending = []
                while self.issued <= min(t + LA_, len(plan) - 1):
                    if plan[self.issued] == "FENCE":
                        if self.issued > t:
                            break
                        self.issued += 1
                        continue
                    self._issue(self.issued, now=(self.issued == t)); self.issued += 1
            b = t % NB_; off = 0; views = []
            for seg in tile:
                nk, ncols = seg[3], seg[5]
                views.append(wbf[b][:, off:off + nk * ncols].rearrange("p (k c) -> p k c", k=nk))
                off += nk * ncols
            return views, self.bfb[b]

    ws = WS()

    def run_tile(segs, units, tcs, evac, wfn=None):
        views, wb = ws.next(segs)
        groups = []
        for ui, unit in enumerate(units):
            for tc in tcs:
                for bi, (si, lc, rhs_fn) in enumerate(unit):
                    ps_ap, ps_b = next_ps()
                    if wfn is not None:
                        ps_ap = ps_ap[:, 0:wfn(tc)]
                    groups.append((ui, tc, si, lc, rhs_fn, ps_ap, ps_b, bi == len(unit) - 1))
        banks = []
        for gi, (ui, tc, si, lc, rhs_fn, ps_ap, ps_b, last) in enumerate(groups):
            nk = segs[si][3]
            extra = [groups[gi + 1][6]] if (gi % 2 == 0 and gi + 1 < len(groups)) else []
            for k in range(nk):
                r_ap, r_b = rhs_fn(k, tc)
                MM(ps_ap, views[si][:, k, lc:lc + 128], r_ap, k == 0, k == nk - 1, [wb, r_b],
                   [ps_b] + (extra if k == 0 else []))
            banks.append((ps_ap, ps_b))
            if last:
                evac(ui, tc, banks)
                banks = []

    DMA(prm[:, :], prm_d, prmb)
    DMA(ident[:, :], ident_d, constb)
    for tc in range(NTC):
        DMA(x[:, :, tc * TCW:(tc + 1) * TCW],
            xT_d[:, tc * TCW:(tc + 1) * TCW].rearrange("(j p) t -> p j t", p=128), xb[tc])
    T.op("dve", lambda h: h.memset(ones_bf[:, :], 1.0), [], [constb])
    T.op("dve", lambda h: h.memset(ones_f[:, :], 1.0), [], [constb])
    for r in range(2):
        for tc in range(NTC):
            i = (r * NTC + tc) % 4
            DMA(tmpf[i], rope_d[r][:, tc * TCW:(tc + 1) * TCW], tmpfb[i])
            ACOPY(ropeT[:, r, tc * TCW:(tc + 1) * TCW], tmpf[i], [tmpfb[i]], [ropeb])
    epsc = prm[:, O_EPS:O_EPS + 1]

    def stats(tc):
        ps_ap, ps_b = next_ps()
        for j in range(8):
            sq, sqb = next_sq()
            ACT(sq, x[:, j, tc * TCW:(tc + 1) * TCW], AF.Square, [xb[tc]], [sqb])
            MM(ps_ap, ones_bf[:, :], sq, j == 0, j == 7, [sqb, constb], [ps_b], inc=True)
        rs = rstd[:, tc * TCW:(tc + 1) * TCW]
        ACT(rs, ps_ap, AF.Sqrt, [ps_b, prmb], [rstdb[tc]], scale=1.0 / D, bias=epsc)
        RECIP(rs, rs, [rstdb[tc]], [rstdb[tc]])

    def apply_norm(gcol, tc, out_fn):
        for j in range(8):
            o_ap, o_b = out_fn(j)
            STT(o_ap, x[:, j, tc * TCW:(tc + 1) * TCW], prm[:, gcol + j:gcol + j + 1],
                rstd[:, tc * TCW:(tc + 1) * TCW], ALU.mult, ALU.mult, [xb[tc], rstdb[tc], prmb], [o_b])

    hT = view(R0, BF16, [8, S]); hTb = [Buf("h%d" % i) for i in range(NTC)]

    def full_norm(gcol):
        for tc in range(NTC):
            stats(tc)
        for tc in range(NTC):
            apply_norm(gcol, tc, lambda j, tc=tc: (hT[:, j, tc * TCW:(tc + 1) * TCW], hTb[tc]))

    def rh_hT(k, tc):
        return hT[:, k, tc * TCW:(tc + 1) * TCW], hTb[tc]

    def x_add_evac(jfn):
        def ev(ui, tc, banks):
            j = jfn(ui)
            xs = x[:, j, tc * TCW:(tc + 1) * TCW]
            TT("dve", xs, xs, banks[0][0], ALU.add, [banks[0][1], xb[tc]], [xb[tc]])
        return ev

    def dump_x():
        for tc in range(NTC):
            DMA(out_d[:, tc * TCW:(tc + 1) * TCW].rearrange("(j p) t -> p j t", p=128),
                x[:, :, tc * TCW:(tc + 1) * TCW], outb, [xb[tc]])

    outb = Buf("out")
    stopped = [False]

    def check_stop(name):
        if debug_stop == name and not stopped[0]:
            stopped[0] = True
            return True
        return False

    def groups_of(n, g):
        out = []; a = 0
        while a < n:
            out.append((a, min(g, n - a))); a += g
        return out

    for l in range(DEPTH):
        if stopped[0]:
            break
        P0 = l * PL
        lam_init = 0.8 - 0.6 * math.exp(-0.3 * l)
        TS("dve", scr[:, 0:1], prm[:, P0 + O_GSUB:P0 + O_GSUB + 1], 1.0 - lam_init, None, ALU.mult, None,
           [prmb], [scrb])
        lamv = prm[:, P0 + O_LAM:P0 + O_LAM + 256]
        TT("dve", tmpf[0][:, 0:64], lamv[:, 0:64], lamv[:, 64:128], ALU.mult, [prmb], [tmpfb[0]])
        TT("dve", tmpf[0][:, 64:128], lamv[:, 128:192], lamv[:, 192:256], ALU.mult, [prmb, tmpfb[0]], [tmpfb[0]])
        T.op("dve", lambda h: h.reduce_sum(out=scr[:, 2:3], in_=tmpf[0][:, 0:64], axis=mybir.AxisListType.X),
             [tmpfb[0], scrb], [scrb])
        T.op("dve", lambda h: h.reduce_sum(out=scr[:, 3:4], in_=tmpf[0][:, 64:128], axis=mybir.AxisListType.X),
             [tmpfb[0], scrb], [scrb])
        ACT(scr[:, 4:6], scr[:, 2:4], AF.Exp, [scrb], [scrb])
        TT("dve", scr[:, 6:7], scr[:, 5:6], scr[:, 4:5], ALU.subtract, [scrb], [scrb])
        TS("dve", scr[:, 1:2], scr[:, 6:7], -lam_init, None, ALU.add, None, [scrb], [scrb])
        gsubp = scr[:, 0:1]; neglam = scr[:, 1:2]

        full_norm(P0 + O_GMIX)
        y = view(R1, F32, [4, S]); yb = [Buf("y%d" % j) for j in range(4)]
        zpad = [view(R2 + i * 4160, BF16, [2080]) for i in range(2)]; zb = [Buf("z0"), Buf("z1")]
        dgm = view(R2 + 8320, BF16, [CK, 128]); dgb = Buf("diag")
        cT = view(R2, BF16, [4, S]); cb = [Buf("c%d" % i) for i in range(NTC)]
        for j in range(4):
            zi = j % 2
            if j < 2:
                T.op("dve", lambda h, zi=zi: h.memset(zpad[zi][:, 0:15], 0.0), [], [zb[zi]])
                T.op("dve", lambda h, zi=zi: h.memset(zpad[zi][:, 15 + S:2080], 0.0), [zb[zi]], [zb[zi]])
            cw0 = P0 + O_CW + j * CK
            for k in range(CK):
                TS("dve", dgm[:, k, :], ident[:, :], prm[:, cw0 + k:cw0 + k + 1], None, ALU.mult, None,
                   [constb, prmb], [dgb])

            def ev_z(ui, tc, banks, zi=zi):
                ACT(tmpf[tc % 2], banks[1][0], AF.Sigmoid, [banks[1][1]], [tmpfb[tc % 2]])
                TT("dve", zpad[zi][:, 15 + tc * TCW:15 + (tc + 1) * TCW], banks[0][0], tmpf[tc % 2], ALU.mult,
                   [banks[0][1], tmpfb[tc % 2], zb[zi]], [zb[zi]])
            run_tile([("w_in", l, 0, 8, 1536 + j * 128, 128), ("w_in", l, 0, 8, 2048 + j * 128, 128)],
                     [[(0, 0, rh_hT), (1, 0, rh_hT)]], range(NTC), ev_z)
            for tc in range(NTC):
                ps_ap, ps_b = next_ps()
                for k in range(CK):
                    MM(ps_ap, dgm[:, k, :], zpad[zi][:, tc * TCW + k:tc * TCW + k + TCW], k == 0, k == CK - 1,
                       [dgb, zb[zi]], [ps_b])
                TS("dve", y[:, j, tc * TCW:(tc + 1) * TCW], ps_ap, prm[:, P0 + O_CB + j:P0 + O_CB + j + 1], None,
                   ALU.add, None, [ps_b, prmb], [yb[j]])
        T.barrier()
        for tc in range(NTC):
            sl = slice(tc * TCW, (tc + 1) * TCW)
            pm, pmb = next_ps(); pq, pqb = next_ps()
            for j in range(4):
                a_, ab_ = next_sq()
                ACOPY(a_, y[:, j, sl], [yb[j]], [ab_])
                MM(pm, ones_bf[:, :], a_, j == 0, j == 3, [ab_, constb], [pmb], inc=True)
                s_, sb_ = next_sq()
                ACT(s_, y[:, j, sl], AF.Square, [yb[j]], [sb_])
                MM(pq, ones_bf[:, :], s_, j == 0, j == 3, [sb_, constb], [pqb], inc=True)
            mean, meanb = tmpf[0], tmpfb[0]; rs_, rsb_ = tmpf[1], tmpfb[1]
            TS("dve", mean, pm, 1.0 / CCH, None, ALU.mult, None, [pmb], [meanb])
            TT("dve", rs_, mean, mean, ALU.mult, [meanb], [rsb_])
            STT(rs_, pq, 1.0 / CCH, rs_, ALU.mult, ALU.subtract, [pqb, rsb_], [rsb_])
            ACT(rs_, rs_, AF.Sqrt, [rsb_, prmb], [rsb_], scale=1.0, bias=epsc)
            RECIP(rs_, rs_, [rsb_], [rsb_])
            for j in range(4):
                t_, tb_ = tmpf[2 + j % 2], tmpfb[2 + j % 2]
                TT("dve", t_, y[:, j, sl], mean, ALU.subtract, [yb[j], meanb], [tb_])
                TT("dve", t_, t_, rs_, ALU.mult, [tb_, rsb_], [tb_])
                ACT(cT[:, j, sl], t_, AF.Silu, [tb_, prmb], [cb[tc]],
                    scale=prm[:, P0 + O_LNG + j:P0 + O_LNG + j + 1], bias=prm[:, P0 + O_LNB + j:P0 + O_LNB + j + 1])
        T.barrier()
        kT = view(R1, BF16, [4, S]); kb = [Buf("k%d" % i) for i in range(NTC)]
        vT = view(R1 + 16384, BF16, [16, 512]); vb = [Buf("v%d" % i) for i in range(NTC)]
        for h_ in range(NH):
            def ev_k(ui, tc, banks, h_=h_):
                sl = slice(tc * TCW, (tc + 1) * TCW)
                TT("dve", tmpf[0], banks[0][0], ropeT[:, 0, sl], ALU.mult, [banks[0][1], ropeb], [tmpfb[0]])
                TT("dve", tmpf[1], banks[1][0], ropeT[:, 1, sl], ALU.mult, [banks[1][1], ropeb], [tmpfb[1]])
                TT("dve", kT[:, h_, sl], tmpf[0], tmpf[1], ALU.add, [tmpfb[0], tmpfb[1]], [kb[tc]])
            run_tile([("w_in", l, 0, 8, 512 + h_ * 128, 128), ("w_qkp", l, 0, 8, 512 + h_ * 128, 128)],
                     [[(0, 0, rh_hT), (1, 0, rh_hT)]], range(NTC), ev_k)
        for half in range(2):
            views, wb = ws.next([("w_in", l, 0, 8, 1024 + half * 256, 256)])
            for tk in range(16):
                ps_ap, ps_b = next_ps()
                for k in range(8):
                    MM(ps_ap[:, 0:256], hT[:, k, tk * 128:(tk + 1) * 128], views[0][:, k, :], k == 0, k == 7,
                       [wb, hTb[tk // 4]], [ps_b])
                ACOPY(vT[:, tk, half * 256:(half + 1) * 256], ps_ap[:, 0:256], [ps_b], [vb[tk // 4]])
        T.barrier()
        hq = view(R0, BF16, [8, TCW]); hqb = Buf("hq")
        qq = view(R0 + 8192, BF16, [4, TCW]); qqb = [Buf("q%d" % i) for i in range(NH)]
        ex = [view(R0 + 12288 + i * 1024, BF16, [TCW]) for i in range(4)]; exb = [Buf("e%d" % i) for i in range(4)]
        on = view(R0 + 16384, BF16, [4, TCW]); onb = [Buf("on%d" % i) for i in range(NH)]
        mg = view(R0 + 20480, BF16, [8, TCW]); mgb = [Buf("m%d" % i) for i in range(8)]
        itc = [0]
        for qc in range(NTC):
            qsl = slice(qc * TCW, (qc + 1) * TCW)
            if qc == 0:
                apply_norm(P0 + O_GMIX, qc, lambda j: (hq[:, j, :], hqb))

            def rh_hq(k, tc):
                return hq[:, k, :], hqb
            for h_ in range(NH):
                def ev_q(ui, tc, banks, h_=h_):
                    TT("dve", tmpf[0], banks[0][0], ropeT[:, 0, qsl], ALU.mult, [banks[0][1], ropeb], [tmpfb[0]])
                    TT("dve", tmpf[1], banks[1][0], ropeT[:, 1, qsl], ALU.mult, [banks[1][1], ropeb], [tmpfb[1]])
                    TT("dve", qq[:, h_, :], tmpf[0], tmpf[1], ALU.add, [tmpfb[0], tmpfb[1]], [qqb[h_]])
                run_tile([("w_in", l, 0, 8, h_ * 128, 128), ("w_qkp", l, 0, 8, h_ * 128, 128)],
                         [[(0, 0, rh_hq), (1, 0, rh_hq)]], [0], ev_q)
            if qc >= 1:
                stats(qc - 1)
            for h_ in range(NH):
                for c_ in range(2):
                    it = itc[0]; itc[0] += 1
                    rows = slice(c_ * 64, (c_ + 1) * 64)
                    pU, pUb = psum[3 + it % 2], psb[3 + it % 2]
                    pD, pDb = psum[5 + it % 2], psb[5 + it % 2]

                    SBK = (0, 1, 2, 7)

                    def S_(kt, extra_r=(), extra_w=()):
                        b_ = SBK[kt % 4]
                        MM(psum[b_], kT[rows, h_, kt * 128:(kt + 1) * 128], qq[rows, h_, :], True, True,
                           [kb[kt // 4], qqb[h_]] + list(extra_r), [psb[b_]] + list(extra_w))
                    S_(0, extra_w=[psb[SBK[1]]]); S_(1)
                    for p_ in range(8):
                        k0, k1 = 2 * p_, 2 * p_ + 1
                        for kt in (k0, k1):
                            ACT(ex[kt % 4], psum[SBK[kt % 4]], AF.Exp, [psb[SBK[kt % 4]]], [exb[kt % 4]],
                                scale=HD ** -0.5)
                        if k0 + 2 < 16:
                            S_(k0 + 2, extra_r=[exb[k0 % 4], exb[k1 % 4], vb[k0 // 4], vb[k1 // 4]],
                               extra_w=[psb[SBK[(k0 + 3) % 4]]])
                            S_(k0 + 3)
                        for kt in (k0, k1):
                            e_, eb_ = ex[kt % 4], exb[kt % 4]
                            MM(pU, vT[:, kt, h_ * 128:(h_ + 1) * 128], e_, kt == 0, kt == 15, [vb[kt // 4], eb_], [pUb])
                            MM(pD, ones_bf[:, :], e_, kt == 0, kt == 15, [constb, eb_], [pDb], inc=True)
                    RECIP(tmpf[2], pD, [pDb], [tmpfb[2]])
                    if c_ == 0:
                        TT("dve", tmpf[0], pU, tmpf[2], ALU.mult, [pUb, tmpfb[2]], [tmpfb[0]])
                    else:
                        TT("dve", tmpf[1], pU, tmpf[2], ALU.mult, [pUb, tmpfb[2]], [tmpfb[1]])
                        STT(tmpf[0], tmpf[1], neglam, tmpf[0], ALU.mult, ALU.add, [tmpfb[1], tmpfb[0], scrb], [tmpfb[0]])
                s_, sb_ = next_sq()
                ACT(s_, tmpf[0], AF.Square, [tmpfb[0]], [sb_])
                stk = 5 + itc[0] % 2
                MM(psum[stk], ones_bf[:, :], s_, True, True, [sb_, constb], [psb[stk]])
                ACT(tmpf[3], psum[stk], AF.Sqrt, [psb[stk], prmb], [tmpfb[3]], scale=1.0 / VD, bias=epsc)
                RECIP(tmpf[3], tmpf[3], [tmpfb[3]], [tmpfb[3]])
                STT(on[:, h_, :], tmpf[0], gsubp, tmpf[3], ALU.mult, ALU.mult, [tmpfb[0], tmpfb[3], scrb], [onb[h_]])

            def rh_on(k, tc):
                return on[:, k, :], onb[k]

            def rh_c(k, tc):
                return cT[:, k, qsl], cb[qc]
            for j in range(8):
                def ev_a(ui, tc, banks, j=j):
                    ACT(tmpf[0], banks[0][0], AF.Sigmoid, [banks[0][1]], [tmpfb[0]])
                    TT("dve", tmpf[1], tmpf[0], banks[1][0], ALU.mult, [tmpfb[0], banks[1][1]], [tmpfb[1]])
                run_tile([("w_in", l, 0, 8, 2560 + j * 128, 128), ("w_ao", l, 0, 4, j * 128, 128)],
                         [[(0, 0, rh_hq), (1, 0, rh_on)]], [0], ev_a)

                def ev_b(ui, tc, banks, j=j):
                    ACT(tmpf[2], banks[0][0], AF.Sigmoid, [banks[0][1]], [tmpfb[2]])
                    TT("dve", tmpf[3], tmpf[2], banks[1][0], ALU.mult, [tmpfb[2], banks[1][1]], [tmpfb[3]])
                    TT("dve", mg[:, j, :], tmpf[1], tmpf[3], ALU.add, [tmpfb[1], tmpfb[3]], [mgb[j]])
                run_tile([("w_in", l, 0, 8, 3584 + j * 128, 128), ("w_co", l, 0, 4, j * 128, 128)],
                         [[(0, 0, rh_hq), (1, 0, rh_c)]], [0], ev_b)

            def rh_m(k, tc):
                return mg[:, k, :], mgb[k]
            if qc + 1 < NTC:
                apply_norm(P0 + O_GMIX, qc + 1, lambda j: (hq[:, j, :], hqb))
            for ct in range(4):
                run_tile([("w_o", l, 0, 8, ct * 256, 256)], [[(0, 0, rh_m)], [(0, 128, rh_m)]], [qc],
                         x_add_evac(lambda ui, ct=ct: ct * 2 + ui))
        stats(NTC - 1)
        T.barrier()
        if check_stop("mix%d" % l):
            break
        act = view(R1, BF16, [8, S]); actb = [Buf("a%d" % i) for i in range(NTC)]

        def rh_act(k, tc):
            return act[:, k, tc * TCW:(tc + 1) * TCW], actb[tc]

        def ffn(gu_name, dn_name, widx, F, down_evac):
            for (g0, gn) in groups_of(F // 128, 8):
                for jj in range(gn):
                    j = g0 + jj

                    def ev_gu(ui, tc, banks, jj=jj):
                        s_, sb__ = tmpf[tc % 2], tmpfb[tc % 2]
                        ACT(s_, banks[0][0], AF.Silu, [banks[0][1]], [sb__])
                        TT("dve", act[:, jj, tc * TCW:(tc + 1) * TCW], s_, banks[1][0], ALU.mult,
                           [sb__, banks[1][1]], [actb[tc]])
                    run_tile([(gu_name, widx, 0, 8, j * 128, 128), (gu_name, widx, 0, 8, F + j * 128, 128)],
                             [[(0, 0, rh_hT), (1, 0, rh_hT)]], range(NTC), ev_gu)
                for ct in range(4):
                    run_tile([(dn_name, widx, g0, gn, ct * 256, 256)], [[(0, 0, rh_act)], [(0, 128, rh_act)]],
                             range(NTC), down_evac(ct))

        if l % 2 == 0:
            for tc in range(NTC):
                apply_norm(P0 + O_GFFN, tc, lambda j, tc=tc: (hT[:, j, tc * TCW:(tc + 1) * TCW], hTb[tc]))
            ffn("w_gu", "w_dn", 0, DFF, lambda ct: x_add_evac(lambda ui, ct=ct: ct * 2 + ui))
        else:
            h32 = view(R1, F32, [8, TCW]); h32b = Buf("h32")
            for tc in range(NTC):
                apply_norm(P0 + O_GFFN, tc, lambda j, tc=tc: (hT[:, j, tc * TCW:(tc + 1) * TCW], hTb[tc]))
                apply_norm(P0 + O_GFFN, tc, lambda j: (h32[:, j, :], h32b))
                for t4 in range(4):
                    tk = tc * 4 + t4
                    ps_ap, ps_b = next_ps()
                    for k in range(8):
                        MM(ps_ap[:, 0:8], h32[:, k, t4 * 128:(t4 + 1) * 128],
                           prm[:, O_WR + k * 8:O_WR + (k + 1) * 8], k == 0, k == 7, [h32b, prmb], [ps_b])
                    lg = rt[:, 0:8]; mx = rt[:, 8:16]; m1 = rt[:, 16:24]; m2 = rt[:, 24:32]
                    TT("dve", lg, ps_ap[:, 0:8], prm[:, O_BR:O_BR + 8], ALU.add, [ps_b, prmb, rtb], [rtb])
                    T.op("dve", lambda h, lg=lg, mx=mx: h.max(out=mx, in_=lg), [rtb], [rtb])
                    TS("dve", m1, lg, rt[:, 8:9], None, ALU.is_equal, None, [rtb], [rtb])
                    TS("dve", m2, lg, rt[:, 9:10], None, ALU.is_equal, None, [rtb], [rtb])
                    TT("dve", rt[:, 32:33], rt[:, 8:9], rt[:, 9:10], ALU.subtract, [rtb], [rtb])
                    ACT(rt[:, 33:34], rt[:, 32:33], AF.Sigmoid, [rtb], [rtb])
                    ACT(rt[:, 34:35], rt[:, 32:33], AF.Sigmoid, [rtb], [rtb], scale=-1.0)
                    TS("dve", comb[:, tk, :], m1, rt[:, 33:34], None, ALU.mult, None, [rtb, combb], [combb])
                    STT(comb[:, tk, :], m2, rt[:, 34:35], comb[:, tk, :], ALU.mult, ALU.add, [rtb, combb], [combb])
                    TT("dve", maskf[:, tk, :], m1, m2, ALU.add, [rtb, mkb], [mkb])
            T.op("dve", lambda h: h.tensor_copy(out=maskb[:, :, :], in_=maskf[:, :, :]), [mkb], [mkb])
            ps_ap, ps_b = next_ps()
            for tk in range(16):
                MM(ps_ap[:, 0:8], ones_bf[:, :], maskb[:, tk, :], tk == 0, tk == 15, [mkb, constb], [ps_b])
            T.op("dve", lambda h: h.tensor_reduce(out=rt[:, 35:36], in_=ps_ap[:, 0:8], axis=mybir.AxisListType.X,
                                                  op=ALU.max), [ps_b, rtb], [rtb])
            fl = rt[:, 40:60]
            TS("dve", fl[:, 0:1], rt[:, 35:36], float(os.environ.get("KTH0", CAP + 0.5)), None, ALU.is_gt, None, [rtb], [rtb])
            flv = fl[:, 1:17].rearrange("p (e c) -> p e c", c=2)
            TS("dve", flv[:, :, 0], ps_ap[:, 0:8], float(os.environ.get("KTH1", 512.5)), None, ALU.is_gt, None, [ps_b, rtb], [rtb])
            TS("dve", flv[:, :, 1], ps_ap[:, 0:8], float(os.environ.get("KTH2", 768.5)), None, ALU.is_gt, None, [ps_b, rtb], [rtb])
            TS("dve", fl[:, 17:20], fl[:, 0:3], 0.0, None, ALU.mult, None, [rtb], [rtb])
            T.op("dve", lambda h: h.memset(zlhs, 0.0), [], [rtb])
            T.op("dve", lambda h: h.tensor_copy(out=flagi[:, :], in_=fl), [rtb], [flagib])
            T.barrier()
            T.load_flag(flagi, flagib, 17)
            ws.fence()
            T.cond_begin()
            cbc = [view(R2 + i * 8192, F32, [S]) for i in range(2)]; cbcb = [Buf("cb0"), Buf("cb1")]
            for e in range(NE):
                ci = e % 2
                for tc in range(NTC):
                    ps_ap, ps_b = next_ps()
                    for t4 in range(4):
                        tk = tc * 4 + t4
                        dg, dgb = tmpf[2 + t4 % 2], tmpfb[2 + t4 % 2]
                        TS("dve", dg[:, 0:128], ident[:, :], comb[:, tk, e:e + 1], None, ALU.mult, None,
                           [constb, combb], [dgb])
                        MM(ps_ap[:, t4 * 128:(t4 + 1) * 128], ones_f[:, :], dg[:, 0:128], True, True,
                           [constb, dgb], [ps_b])
                    ACOPY(cbc[ci][:, tc * TCW:(tc + 1) * TCW], ps_ap, [ps_b], [cbcb[ci]])

                def dn_evac(ct, ci=ci):
                    def ev(ui, tc, banks):
                        j = ct * 2 + ui
                        t_, tb_ = tmpf[2 + (ui + tc) % 2], tmpfb[2 + (ui + tc) % 2]
                        TT("dve", t_, banks[0][0], cbc[ci][:, tc * TCW:(tc + 1) * TCW], ALU.mult,
                           [banks[0][1], cbcb[ci]], [tb_])
                        xs = x[:, j, tc * TCW:(tc + 1) * TCW]
                        TT("dve", xs, xs, t_, ALU.add, [tb_, xb[tc]], [xb[tc]])
                    return ev
                ffn("we_gu", "we_dn", e, DEX, dn_evac)

            ws.fence()
            T.cond_else()
            assert l == DEPTH - 1
            NSB = CAP // 128
            chunks = [(0, 512, None), (512, 256, 0), (768, 256, 1)]
            plo = Buf("P_act_lo"); phi = Buf("act_hi"); heb = Buf("hE_eo"); hTMb = Buf("hTM"); cstb = Buf("cst"); r4b = Buf("rhs4")
            sib = Buf("sinfo"); posb = Buf("pos"); hscrb = Buf("hscr")
            actS = view(0, BF16, [28, CAP]); Pm = view(0, BF16, [16, CAP])
            hTM = view(32768, BF16, [16, 1024])
            hE = view(65536, BF16, [8, CAP]); eo = view(65536, BF16, [NSB, 1024])
            tS = [view(81920 + i * 2048, F32, [512]) for i in range(2)]; tSb = [Buf("tS0"), Buf("tS1")]
            iota = view(87552, F32, [1024])
            ropeflat = ropeT[:, :, :].rearrange("p a b -> p (a b)")
            PT = [rstd[:, :].bitcast(BF16).rearrange("p (s t) -> p s t", s=NSB),
                  ropeflat.rearrange("p (s t) -> p s t", s=NSB)]
            PTb = [Buf("PT0"), Buf("PT1")]
            triu_b = view(91904, BF16, [128])
            rhs4 = view(92160, BF16, [16, 4])
            identb = view(92288, BF16, [128])
            DMA(iota, cst_d[:, 0:1024], cstb)
            DMA(tS[1][:, 0:128], cst_d[:, 1024:1152], tSb[1])
            DMA(tS[0][:, 0:32], cst_d[:, 1152:1184], tSb[0])
            ACOPY(triu_b, tS[1][:, 0:128], [tSb[1]], [cstb])
            ACOPY(identb, ident[:, :], [constb, cstb], [cstb])
            ACOPY(rhs4[:, :, 0:2], tS[0][:, 0:32].rearrange("p (t c) -> p t c", c=2), [tSb[0]], [r4b])
            for tk in range(16):
                for kq in range(2):
                    ps_ap, ps_b = next_ps()
                    pbf = ps_ap.bitcast(BF16)
                    for k4 in range(4):
                        k = kq * 4 + k4
                        o_ = pbf[:, k4 * 128:(k4 + 1) * 128]; i_ = hT[:, k, tk * 128:(tk + 1) * 128]
                        T.op("pe", lambda h, o_=o_, i_=i_: h.transpose(o_, i_, identb), [hTb[tk // 4], cstb], [ps_b])
                    ACOPY(hTM[:, tk, kq * 512:(kq + 1) * 512], pbf[:, 0:512], [ps_b], [hTMb])
            hflat = hTM.rearrange("p a b -> p (a b)")
            DMA(hscr_d, hflat, hscrb, [hTMb])
            for tk in range(16):
                ps_ap, ps_b = next_ps()
                MM(ps_ap[:, 0:8], triu_b, maskb[:, tk, :], True, tk == 0, [cstb, mkb], [ps_b])
                for t2 in range(tk):
                    MM(ps_ap[:, 0:8], ones_bf[:, :], maskb[:, t2, :], False, t2 == tk - 1, [constb, mkb], [ps_b])
                ACOPY(posf[:, tk, :], ps_ap[:, 0:8], [ps_b], [posb])
            T.barrier()

            def cond_chain(e, items):
                def rec(i):
                    if i >= len(items):
                        return
                    fi, fn = items[i]
                    if fi is None or not USE_SKIP:
                        fn(); rec(i + 1)
                    else:
                        T.skip_begin(1 + e * 2 + fi)
                        fn(); rec(i + 1)
                        T.skip_end()
                rec(0)

            for e in range(NE):
                if e > 0:
                    T.dma([lambda h: h.dma_start(out=hflat, in_=hscr_d)], hTMb, [hscrb], also=[phi])
                def prep_ops(e2):
                    ops = [lambda: ACOPY(rhs4[:, :, 2], comb[:, :, e2], [combb], [r4b]),
                           lambda: TT("dve", rhs4[:, :, 3], comb[:, :, e2], rhs4[:, :, 2], ALU.subtract,
                                      [combb, r4b], [r4b])]
                    for tk in range(16):
                        ops.append(lambda tk=tk: TS("dve", Pm[:, tk, :], iota, posf[:, tk, e2:e2 + 1],
                                                    maskf[:, tk, e2:e2 + 1], ALU.is_equal, ALU.mult,
                                                    [cstb, posb, mkb], [plo]))
                    return ops
                if e == 0:
                    for f_ in prep_ops(0):
                        f_()
                pend = prep_ops(e + 1) if e + 1 < NE else []
                its = []
                for (c0, W, fi) in chunks:
                    def g_(c0=c0, W=W):
                        pbs = [next_ps() for _ in range(8)]
                        for k in range(8):
                            ps_ap, ps_b = pbs[k]
                            for tk in range(16):
                                MM(ps_ap[:, 0:W], hTM[:, tk, k * 128:(k + 1) * 128], Pm[:, tk, c0:c0 + W],
                                   tk == 0, tk == 15, [hTMb, plo],
                                   [ps_b] + ([pbs[k + 1][1]] if (tk == 0 and k % 2 == 0) else []))
                            if k % 2 == 0:
                                ACOPY(hE[:, k, c0:c0 + W], ps_ap[:, 0:W], [ps_b], [heb])
                            else:
                                T.op("dve", lambda h, o_=hE[:, k, c0:c0 + W], i_=ps_ap[:, 0:W]:
                                     h.tensor_copy(out=o_, in_=i_), [ps_b], [heb])
                    its.append((fi, g_))
                cond_chain(e, its)
                for sb_ in range(NSB):
                    ps_ap, ps_b = next_ps()
                    for tk in range(16):
                        MM(ps_ap[:, 0:4], Pm[:, tk, sb_ * 128:(sb_ + 1) * 128], rhs4[:, tk, :], tk == 0, tk == 15,
                           [plo, r4b], [ps_b])
                    ACOPY(sinfo[:, sb_, :], ps_ap[:, 0:4], [ps_b], [sib])
                TT("dve", tokw[:, 0, :], sinfo[:, :, 0], sinfo[:, :, 1], ALU.add, [sib], [sib])
                TT("dve", tokw[:, 1, :], sinfo[:, :, 2], sinfo[:, :, 3], ALU.add, [sib], [sib])

                def build_pt(tc):
                    pt, ptb = PT[tc % 2], PTb[tc % 2]
                    TS("dve", tokw[:, 2, :], tokw[:, 0, :], float(-tc * TCW), None, ALU.add, None, [sib], [sib])
                    for sb_ in range(NSB):
                        TS("dve", pt[:, sb_, :], iota[:, 0:TCW], tokw[:, 2, sb_:sb_ + 1], None, ALU.is_equal, None,
                           [cstb, sib], [ptb])
                build_pt(0); build_pt(1)
                for j in range(DEX // 128):
                    views, wb = ws.next([("we_gu", e, 0, 8, j * 128, 128), ("we_gu", e, 0, 8, DEX + j * 128, 128)])
                    its = []
                    for ci, (c0, W, fi) in enumerate(chunks):
                        def gu_(c0=c0, W=W, ci=ci, j=j, views=views, wb=wb):
                            pg, pgb = next_ps(); pu, pub = next_ps()
                            for (si, pp_, ppb_) in ((0, pg, pgb), (1, pu, pub)):
                                for k in range(8):
                                    MM(pp_[:, 0:W], views[si][:, k, 0:128], hE[:, k, c0:c0 + W], k == 0, k == 7,
                                       [wb, heb], [ppb_] + ([pub] if (si == 0 and k == 0) else []))
                            t_, tb_ = tS[ci % 2], tSb[ci % 2]
                            ACT(t_[:, 0:W], pg[:, 0:W], AF.Silu, [pgb], [tb_])
                            TT("dve", actS[:, j, c0:c0 + W], t_[:, 0:W], pu[:, 0:W], ALU.mult, [tb_, pub], [plo, phi])
                        its.append((fi, gu_))
                    cond_chain(e, its)
                sbg = [(0, 4, None), (4, 2, 0), (6, 2, 1)]
                for dh in range(2):
                    for kg in range(7):
                        views, wb = ws.next([("we_dn", e, kg * 4, 4, dh * 512, 512)])
                        its = []
                        for (s0, sn, fi) in sbg:
                            def dn_(s0=s0, sn=sn, kg=kg, views=views, wb=wb):
                                for sb_ in range(s0, s0 + sn):
                                    for k4 in range(4):
                                        MM(psum[sb_], actS[:, kg * 4 + k4, sb_ * 128:(sb_ + 1) * 128],
                                           views[0][:, k4, :], kg == 0 and k4 == 0, kg == 6 and k4 == 3,
                                           [wb, plo, phi], [psb[sb_]], inc=(sb_ == s0 + sn - 1 and k4 == 3))
                            its.append((fi, dn_))
                        cond_chain(e, its)
                    its = []
                    for (s0, sn, fi) in sbg:
                        def de_(s0=s0, sn=sn, dh=dh):
                            for sb_ in range(s0, s0 + sn):
                                if sb_ % 2 == 0:
                                    TS("dve", eo[:, sb_, dh * 512:(dh + 1) * 512], psum[sb_], tokw[:, 1, sb_:sb_ + 1],
                                       None, ALU.mult, None, [psb[sb_], sib], [heb])
                                else:
                                    ACT(eo[:, sb_, dh * 512:(dh + 1) * 512], psum[sb_], AF.Copy, [psb[sb_], sib], [heb],
                                        scale=tokw[:, 1, sb_:sb_ + 1])
                        its.append((fi, de_))
                    cond_chain(e, its)
                for tc in range(NTC):
                    pt, ptb = PT[tc % 2], PTb[tc % 2]
                    if 1 <= tc <= 2:
                        build_pt(tc + 1)
                    for j in range(8):
                        ps_ap, ps_b = next_ps()
                        its = []
                        for (s0, sn, fi) in sbg:
                            def sc_(s0=s0, sn=sn, j=j, ps_ap=ps_ap, ps_b=ps_b, last=(s0 + sn == NSB), st=(s0 == 0)):
                                for sb_ in range(s0, s0 + sn):
                                    MM(ps_ap, eo[:, sb_, j * 128:(j + 1) * 128], pt[:, sb_, :], st and sb_ == s0,
                                       False, [heb, ptb], [ps_b], inc=(sb_ == s0 + sn - 1))
                            its.append((fi, sc_))
                        cond_chain(e, its)
                        MM(ps_ap, zlhs, pt[:, 0, :], False, True, [cstb, ptb], [ps_b])
                        xs = x[:, j, tc * TCW:(tc + 1) * TCW]
                        TT("dve", xs, xs, ps_ap, ALU.add, [ps_b, xb[tc]], [xb[tc]])
                        if pend:
                            pend.pop(0)()
                while pend:
                    pend.pop(0)()
            ws.fence()
            T.cond_end()
        T.barrier()
        if check_stop("ffn%d" % l):
            break
        full_norm(P0 + O_GPLE)
        pTb = view(R1, BF16, [2, S]); pb = [Buf("p%d" % i) for i in range(NTC)]
        pst = [view(R1 + 8192 + i * 4096, F32, [2, TCW]) for i in range(2)]; pstb = [Buf("ps0"), Buf("ps1")]
        for tc in range(NTC):
            i = tc % 2
            DMA(pst[i], pT_d[l][:, tc * TCW:(tc + 1) * TCW].rearrange("(j p) t -> p j t", p=128), pstb[i])
            ACOPY(pTb[:, :, tc * TCW:(tc + 1) * TCW], pst[i], [pstb[i]], [pb[tc]])

        def rh_p(k, tc):
            return pTb[:, k, tc * TCW:(tc + 1) * TCW], pb[tc]
        for j in range(8):
            def ev_p(ui, tc, banks, j=j):
                s_, sb__ = tmpf[tc % 2], tmpfb[tc % 2]
                t_, tb_ = tmpf[2 + tc % 2], tmpfb[2 + tc % 2]
                ACT(s_, banks[0][0], AF.Sigmoid, [banks[0][1]], [sb__])
                TT("dve", t_, s_, banks[1][0], ALU.mult, [sb__, banks[1][1]], [tb_])
                xs = x[:, j, tc * TCW:(tc + 1) * TCW]
                TT("dve", xs, xs, t_, ALU.add, [tb_, xb[tc]], [xb[tc]])
            run_tile([("w_pg", l, 0, 8, j * 128, 128), ("w_pp", l, 0, 2, j * 128, 128)],
                     [[(0, 0, rh_hT), (1, 0, rh_p)]], range(NTC), ev_p)
        T.barrier()
        if check_stop("ple%d" % l):
            break

    if not stopped[0]:
        for tc in range(NTC):
            stats(tc)
            apply_norm(O_GFIN, tc, lambda j, tc=tc: (x[:, j, tc * TCW:(tc + 1) * TCW], xb[tc]))
    dump_x()
    fin = [(outb.sem, outb.cnt)]
    T.prog["sp"].append((fin, None, None, 0))
    if plan is None:
        es.close()
        return None, ws.rec
    assert ws.i == len(plan)
    print('prog sizes', {e: len(p) for e, p in T.prog.items()}, 'nsem', len(T.semh))
    assert T.simulate(True), 'deadlock in semaphore program (if)'
    assert T.simulate(False, True), 'deadlock in semaphore program (else, chunks on)'
    assert T.simulate(False, False), 'deadlock in semaphore program (else, chunks off)'
    T.emit()
    es.close()
    return nc, ws.rec


def _prep(inputs):
    g = lambda k: np.asarray(inputs[k], dtype=np.float32)
    x = g("x"); p = g("p")
    w_in = g("w_in")
    idx = np.arange(512)
    partner = (idx // 64) * 64 + ((idx % 64) + 32) % 64
    w_qkp = np.ascontiguousarray(np.concatenate([w_in[:, :, partner], w_in[:, :, 512 + partner]], axis=2))
    prm = np.zeros((128, NP), np.float32)

    def col8(v):
        return v.reshape(-1, 128).T

    for l in range(DEPTH):
        P0 = l * PL
        prm[:, P0 + O_GMIX:P0 + O_GMIX + 8] = col8(g("g_mix")[l])
        prm[:, P0 + O_GFFN:P0 + O_GFFN + 8] = col8(g("g_ffn")[l])
        prm[:, P0 + O_GPLE:P0 + O_GPLE + 8] = col8(g("g_ple")[l])
        prm[:, P0 + O_GSUB] = g("g_subln")[l]
        prm[:, P0 + O_CB:P0 + O_CB + 4] = col8(g("conv_b")[l])
        prm[:, P0 + O_LNG:P0 + O_LNG + 4] = col8(g("conv_ln_g")[l])
        prm[:, P0 + O_LNB:P0 + O_LNB + 4] = col8(g("conv_ln_b")[l])
        cw = g("conv_w")[l]
        for j in range(4):
            prm[:, P0 + O_CW + j * CK:P0 + O_CW + (j + 1) * CK] = cw[:, j * 128:(j + 1) * 128].T
        prm[:, P0 + O_LAM:P0 + O_LAM + 256] = np.broadcast_to(g("lam")[l].reshape(1, 256), (128, 256))
    prm[:, O_GFIN:O_GFIN + 8] = col8(g("g_final"))
    prm[:, O_BR:O_BR + 8] = np.broadcast_to(g("b_router")[0].reshape(1, 8), (128, 8))
    wr = g("w_router")[0]
    prm[:, O_WR:O_WR + 64] = wr.reshape(8, 128, 8).transpose(1, 0, 2).reshape(128, 64)
    prm[:, O_EPS] = EPS
    inv_freq = (10000.0 ** (-np.arange(0, HD, 2, dtype=np.float32) / HD)).astype(np.float32)
    ang = (np.arange(S, dtype=np.float32)[:, None] * inv_freq[None, :]).astype(np.float32)
    cos = np.cos(ang).astype(np.float32); sin = np.sin(ang).astype(np.float32)
    pp = np.arange(128)
    sign = np.where((pp % 64) < 32, -1.0, 1.0).astype(np.float32)
    rope = np.stack([cos[:, pp % 32].T, sin[:, pp % 32].T * sign[:, None]]).astype(np.float32)
    shared = {
        "w_in": w_in, "w_qkp": w_qkp, "w_attn_out": g("w_attn_out"), "w_conv_out": g("w_conv_out"),
        "w_o": g("w_o"), "w_ff_gu": g("w_ff_gu"), "w_ff_down": g("w_ff_down"),
        "we_gu": g("we_gu")[0], "we_down": g("we_down")[0], "w_ple_gate": g("w_ple_gate"),
        "w_ple_proj": g("w_ple_proj"), "prm": prm, "rope": np.ascontiguousarray(rope),
        "ident": np.eye(128, dtype=np.float32),
    }
    cst = np.zeros((128, 1184), np.float32)
    cst[:, 0:1024] = np.arange(1024, dtype=np.float32)[None, :]
    cst[:, 1024:1152] = np.triu(np.ones((128, 128), np.float32), k=1)
    tokc = np.zeros((128, 16, 2), np.float32)
    tokc[:, :, 0] = np.arange(128, dtype=np.float32)[:, None]
    tokc[:, :, 1] = (np.arange(16, dtype=np.float32) * 128.0)[None, :]
    cst[:, 1152:1184] = tokc.reshape(128, 32)
    shared["cst"] = cst
    in_maps = []
    for b in range(8):
        m = dict(shared)
        m["xT"] = np.ascontiguousarray(x[b].T)
        m["pT"] = np.ascontiguousarray(p[:, b].transpose(0, 2, 1))
        in_maps.append(m)
    return in_maps


def kernel(**inputs):
    dbg = os.environ.get("KDEBUG") or None
    _, plan = build(None, dbg)
    nc, _ = build(plan, dbg)
    in_maps = _prep(inputs)
    res = run_bass_kernel_spmd(nc, in_maps, core_ids=list(range(8)))
    out = np.stack([np.asarray(r["outT"]).T for r in res.results]).astype(np.float32)
    return out
```

```python
import math
import os
import numpy as np
from contextlib import ExitStack
import concourse.bass as bass
import concourse.mybir as mybir
from concourse.bass_utils import run_bass_kernel_spmd

F32 = mybir.dt.float32
BF16 = mybir.dt.bfloat16
AF = mybir.ActivationFunctionType
ALU = mybir.AluOpType

D = 1024; S = 2048; DEPTH = 2; NH = 4; HD = 64; VD = 128; CCH = 512; CK = 31
DFF = 2816; NE = 8; DEX = 3584; PLE = 256; INC = 4608
EPS = 1e-6
NTC = 4; TCW = 512
PL = 417
O_GMIX, O_GFFN, O_GPLE, O_GSUB, O_CB, O_LNG, O_LNB, O_CW, O_LAM = 0, 8, 16, 24, 25, 29, 33, 37, 161
O_GFIN = 2 * PL; O_BR = O_GFIN + 8; O_WR = O_BR + 8; O_EPS = O_WR + 64; NP = O_EPS + 1
NS_, NB_, LA_ = 2, 3, 2
HOIST = os.environ.get("KNOHOIST") is None


class Buf:
    __slots__ = ("name", "w", "r", "sem", "cnt")
    REG = []

    def __init__(self, name=""):
        self.name = name; self.w = None; self.r = {}; self.sem = None; self.cnt = 0
        Buf.REG.append(self)


class Tracker:
    def __init__(self, nc, es):
        self.nc = nc; self.es = es
        self.engs = ["pe", "act", "dve", "pool", "sp"]
        self.semh = {}
        for e in ["pe", "act", "dve", "pool"]:
            self.semh[e] = es.enter_context(nc.semaphore("s_" + e))
        self.cnt = {e: 0 for e in ["pe", "act", "dve", "pool"]}
        self.seen = {e: {} for e in self.engs}
        self.prog = {e: [] for e in self.engs}
        self.dbufs = []
        self.regs = {}

    def load_flag(self, flag_ap, flagb, n):
        for e in self.engs:
            w = self._waits(e, [flagb], [])
            for i in range(n):
                def f(h, e=e, i=i):
                    r = h.alloc_register("flag_%s_%d" % (e, i))
                    ins = h.reg_load(r, flag_ap[0:1, i:i + 1])
                    self.regs[(e, i)] = h.snap(r)
                    return ins
                self.prog[e].append((w if i == 0 else [], f, None, 0))

    def skip_begin(self, fidx):
        if not hasattr(self, "_sstack"):
            self._sstack = []
        self._sstack.append((dict(self.cnt), {e: dict(d) for e, d in self.seen.items()}))
        for e in ("pe", "act", "dve"):
            self.prog[e].append(("IFR", fidx, None, 0))

    def skip_end(self):
        s_cnt, s_seen = self._sstack.pop()
        for e in ("pe", "act", "dve"):
            d = self.cnt[e] - s_cnt[e]
            self.prog[e].append(("ELSE", None, None, 0))
            if d > 0:
                self.prog[e].append(([], "INC", e, d))
            self.prog[e].append(("END", None, None, 0))
        assert self.cnt["pool"] == s_cnt["pool"]
        self.seen = s_seen

    def _dsem(self, buf):
        if buf.sem is None:
            key = "d%d" % len(self.dbufs)
            buf.sem = key
            self.dbufs.append(buf)
            self.semh[key] = self.es.enter_context(self.nc.semaphore(key))
        return buf.sem

    def _waits(self, eng, reads, writes):
        ev = {}
        for b in reads:
            if b.w is not None and ev.get(b.w[0], 0) < b.w[1]:
                ev[b.w[0]] = b.w[1]
        for b in writes:
            if b.w is not None and ev.get(b.w[0], 0) < b.w[1]:
                ev[b.w[0]] = b.w[1]
            for k, v in b.r.items():
                if ev.get(k, 0) < v:
                    ev[k] = v
        out = []
        for k, v in ev.items():
            if eng == "pe" and k == "pe":
                continue
            if self.seen[eng].get(k, 0) >= v:
                continue
            self.seen[eng][k] = v
            out.append((k, v))
        return out

    def op(self, eng, fn, reads=(), writes=(), inc=True):
        w = self._waits(eng, reads, writes)
        if inc:
            self.cnt[eng] += 1
            ev = (eng, self.cnt[eng])
        else:
            ev = (eng, self.cnt[eng] + 1)
        self.prog[eng].append((w, fn, eng if inc else None, 1))
        for b in reads:
            if b.r.get(ev[0], 0) < ev[1]:
                b.r[ev[0]] = ev[1]
        for b in writes:
            b.w = ev; b.r = {}

    def dma(self, fns, dst, reads=(), q="sp", also=()):
        w = self._waits(q, reads, [dst] + list(also))
        key = self._dsem(dst)
        for i, fn in enumerate(fns):
            self.prog[q].append((w if i == 0 else [], fn, key, 16))
        dst.cnt += 16 * len(fns)
        ev = (key, dst.cnt)
        for b in reads:
            if b.r.get(key, 0) < ev[1]:
                b.r[key] = ev[1]
        dst.w = ev; dst.r = {}
        for b in also:
            b.w = ev; b.r = {}
        return ev

    def barrier(self):
        tg = [(e, self.cnt[e]) for e in ["pe", "act", "dve", "pool"] if self.cnt[e] > 0]
        tg += [(b.sem, b.cnt) for b in self.dbufs]
        for e in self.engs:
            w = []
            for k, v in tg:
                if e == "pe" and k == "pe":
                    continue
                if self.seen[e].get(k, 0) >= v:
                    continue
                self.seen[e][k] = v
                w.append((k, v))
            if w:
                self.prog[e].append((w, None, None, 0))

    def _snap(self):
        return (dict(self.cnt), {e: dict(d) for e, d in self.seen.items()},
                [(b, b.w, dict(b.r), b.cnt) for b in Buf.REG])

    def _restore(self, st):
        self.cnt = dict(st[0]); self.seen = {e: dict(d) for e, d in st[1].items()}
        for (b, w, r, c) in st[2]:
            b.w = w; b.r = dict(r); b.cnt = c

    def _endstate(self):
        d = dict(self.cnt)
        for b in self.dbufs:
            d[b.sem] = b.cnt
        return d

    def cond_begin(self):
        self._c0 = self._snap()
        self._ndb0 = len(self.dbufs)
        for e in self.engs:
            self.prog[e].append(("IF", None, None, 0))

    def cond_else(self):
        self.barrier()
        self._e1 = self._endstate()
        self._else_idx = {e: len(self.prog[e]) for e in self.engs}
        for e in self.engs:
            self.prog[e].append(("ELSE", None, None, 0))
        self._restore(self._c0)

    def cond_end(self):
        self.barrier()
        e2 = self._endstate(); e1 = self._e1
        keys = set(e1) | set(e2)
        own = {"pe": "pe", "act": "act", "dve": "dve", "pool": "pool"}
        for k in keys:
            a = e1.get(k, 0); b_ = e2.get(k, 0); tgt = max(a, b_)
            eng = own.get(k, "sp")
            if tgt > a:
                self.prog[eng].insert(self._else_idx[eng], ([], "INC", k, tgt - a))
                for e in self.engs:
                    if e != eng:
                        pass
                self._else_idx[eng] += 1
            if tgt > b_:
                self.prog[eng].append(([], "INC", k, tgt - b_))
            if k in self.cnt:
                self.cnt[k] = tgt
        for b in self.dbufs:
            b.cnt = max(e1.get(b.sem, 0), e2.get(b.sem, 0))
        for e in self.engs:
            self.prog[e].append(("END", None, None, 0))
            for k in keys:
                self.seen[e][k] = max(e1.get(k, 0), e2.get(k, 0))
        for b in Buf.REG:
            b.w = None; b.r = {}

    def simulate(self, take_if=True, take_ifr=True):
        sem = {}
        ptr = {e: 0 for e in self.engs}
        mode = {e: 0 for e in self.engs}
        inner = {e: [] for e in self.engs}
        total = sum(len(p) for p in self.prog.values())
        done = 0
        while True:
            prog_made = False
            for e in self.engs:
                p = self.prog[e]
                while ptr[e] < len(p):
                    w, fn, sk, amt = p[ptr[e]]
                    if w == "IFR" or (inner[e] and w in ("ELSE", "END")):
                        if w == "IFR":
                            inner[e].append(1)
                        elif w == "ELSE":
                            inner[e][-1] = 2
                        else:
                            inner[e].pop()
                        ptr[e] += 1; done += 1; prog_made = True
                        continue
                    if w in ("IF", "ELSE", "END"):
                        mode[e] = {"IF": 1, "ELSE": 2, "END": 0}[w]
                        ptr[e] += 1; done += 1; prog_made = True
                        continue
                    if (mode[e] == 1 and not take_if) or (mode[e] == 2 and take_if) or \
                            any((m_ == 1 and not take_ifr) or (m_ == 2 and take_ifr) for m_ in inner[e]):
                        ptr[e] += 1; done += 1; prog_made = True
                        continue
                    if any(sem.get(k, 0) < v for k, v in w):
                        break
                    if sk is not None:
                        sem[sk] = sem.get(sk, 0) + amt
                    ptr[e] += 1; done += 1; prog_made = True
            if done == total:
                return True
            if not prog_made:
                for e in self.engs:
                    if ptr[e] < len(self.prog[e]):
                        w = self.prog[e][ptr[e]][0]
                        print("DEADLOCK", e, ptr[e], len(self.prog[e]), [(k, v, sem.get(k, 0)) for k, v in w if sem.get(k, 0) < v])
                return False

    def emit(self):
        nc = self.nc
        hmap = {"pe": "tensor", "act": "scalar", "dve": "vector", "pool": "gpsimd", "sp": "sync"}
        semh = self.semh
        with nc.Block() as block:
            for e, attr in hmap.items():
                prog = self.prog[e]

                def body(h, prog=prog, e=e):
                    stack = []
                    n = len(prog); idx = 0
                    while idx < n:
                        w, fn, sk, amt = prog[idx]
                        if w == "IF":
                            cm = h.If(self.regs[(e, 0)] > 0); cm.__enter__(); stack.append([cm, None])
                            idx += 1; continue
                        if w == "IFR":
                            cm = h.If(self.regs[(e, fn)] > 0); cm.__enter__(); stack.append([cm, None])
                            idx += 1; continue
                        if w == "ELSE":
                            ent = stack.pop(); ent[0].__exit__(None, None, None)
                            cm = h.Else(); cm.__enter__(); stack.append([cm, None])
                            idx += 1
                            if ent[1] is not None:
                                hfn, hsk, hamt = ent[1]
                                d = 0
                                if idx < n and prog[idx][1] == "INC" and prog[idx][2] == e:
                                    d = prog[idx][3]; idx += 1
                                ins = hfn(h)
                                tot = (hamt if hsk is not None else 0) + d
                                if tot > 0:
                                    ins.then_inc(semh[e], tot)
                            continue
                        if w == "END":
                            stack.pop()[0].__exit__(None, None, None)
                            idx += 1; continue
                        for (k, v) in w:
                            h.wait_ge(semh[k], v)
                        if fn is None:
                            idx += 1; continue
                        if fn == "INC":
                            if sk == e:
                                h.drain().then_inc(semh[sk], amt)
                            else:
                                h.sem_inc(semh[sk], amt)
                            idx += 1; continue
                        nxt = prog[idx + 1] if idx + 1 < n else None
                        if HOIST and nxt is not None and nxt[0] == "IFR" and (sk is None or sk == e):
                            cm = h.If(self.regs[(e, nxt[1])] > 0); cm.__enter__(); stack.append([cm, (fn, sk, amt)])
                            ins = fn(h)
                            if sk is not None:
                                ins.then_inc(semh[sk], amt)
                            idx += 2; continue
                        ins = fn(h)
                        if sk is not None:
                            ins.then_inc(semh[sk], amt)
                        idx += 1
                getattr(block, attr)(body)


def build(plan, debug_stop=None):
    nc = bass.Bass("TRN2", target_bir_lowering=False)
    es = ExitStack()
    Buf.REG = []
    T = Tracker(nc, es)

    def din(name, shape):
        return nc.dram_tensor(name, shape, F32, kind="ExternalInput").ap()

    xT_d = din("xT", [D, S]); pT_d = din("pT", [DEPTH, PLE, S])
    WD = {
        "w_in": din("w_in", [DEPTH, D, INC]), "w_qkp": din("w_qkp", [DEPTH, D, 1024]),
        "w_ao": din("w_attn_out", [DEPTH, 512, D]), "w_co": din("w_conv_out", [DEPTH, 512, D]),
        "w_o": din("w_o", [DEPTH, D, D]), "w_gu": din("w_ff_gu", [1, D, 2 * DFF]),
        "w_dn": din("w_ff_down", [1, DFF, D]), "we_gu": din("we_gu", [NE, D, 2 * DEX]),
        "we_dn": din("we_down", [NE, DEX, D]), "w_pg": din("w_ple_gate", [DEPTH, D, D]),
        "w_pp": din("w_ple_proj", [DEPTH, PLE, D]),
    }
    prm_d = din("prm", [128, NP]); rope_d = din("rope", [2, 128, S]); ident_d = din("ident", [128, 128])
    out_d = nc.dram_tensor("outT", [D, S], F32, kind="ExternalOutput").ap()
    cst_d = din("cst", [128, 1184])
    flag_d = nc.dram_tensor("flag_scr", [1, 32], mybir.dt.int32, kind="Internal").ap()

    def sb(name, shape, dt):
        return es.enter_context(nc.sbuf_tensor("sb_" + name, shape, dt))

    x = sb("x", [128, 8, S], F32); xb = [Buf("x%d" % i) for i in range(NTC)]
    wst = [sb("wst%d" % i, [128, 2048], F32) for i in range(NS_)]
    wbf = [sb("wbf%d" % i, [128, 2048], BF16) for i in range(NB_)]
    rstd = sb("rstd", [128, S], F32); rstdb = [Buf("rs%d" % i) for i in range(NTC)]
    prm = sb("prm", [128, NP], F32); prmb = Buf("prm")
    ropeT = sb("ropeT", [128, 2, S], BF16); ropeb = Buf("rope")
    ones_bf = sb("ones_bf", [128, 128], BF16); ones_f = sb("ones_f", [128, 128], F32)
    ident = sb("ident", [128, 128], F32); constb = Buf("const")
    scr = sb("scr", [128, 16], F32); scrb = Buf("scr")
    comb = sb("comb", [128, 16, 8], F32); combb = Buf("comb")
    rt = sb("rt", [128, 64], F32); rtb = Buf("rt")
    ARENA_B = 93440
    arena = sb("arena", [128, ARENA_B // 2], BF16)

    def view(off, dt, shape):
        n = 1
        for s_ in shape:
            n *= s_
        esz = 4 if dt == F32 else 2
        a = arena[:, off // 2: off // 2 + n * esz // 2]
        if dt == F32:
            a = a.bitcast(F32)
        if len(shape) == 2:
            a = a.rearrange("p (a b) -> p a b", a=shape[0])
        return a

    psum = [es.enter_context(nc.psum_tensor("ps%d" % i, [128, 512], F32))[:, :] for i in range(8)]
    psb = [Buf("ps%d" % i) for i in range(8)]
    psi = [0]

    def next_ps():
        i = psi[0] % 8; psi[0] += 1
        return psum[i], psb[i]

    R0, R1, R2, RT = 0, 32768, 65536, 82176
    tmpf = [view(RT + i * 2048, F32, [512]) for i in range(4)]; tmpfb = [Buf("tf%d" % i) for i in range(4)]
    sqt = [view(RT + 8192 + i * 1024, BF16, [512]) for i in range(3)]; sqtb = [Buf("sq%d" % i) for i in range(3)]
    sqi = [0]

    def next_sq():
        i = sqi[0] % 3; sqi[0] += 1
        return sqt[i], sqtb[i]

    CAP = 1024
    USE_SKIP = os.environ.get("KNOSKIP") is None
    maskf = view(86016, F32, [16, 8]); posf = view(86528, F32, [16, 8])
    tokw = view(87040, F32, [3, 8]); sinfo = view(87168, F32, [8, 4])
    maskb = view(87296, BF16, [16, 8]); mkb = Buf("mask")
    zlhs = view(91648, BF16, [128])
    hscr_d = nc.dram_tensor("h_scr", [128, 16 * 1024], BF16, kind="Internal").ap()
    flagi = sb("flagi", [128, 20], mybir.dt.int32); flagib = Buf("flagi")

    def ACT(out, in_, func, reads, writes, scale=None, bias=None):
        kw = {}
        if scale is not None:
            kw["scale"] = scale
        if bias is not None:
            kw["bias"] = bias
        T.op("act", lambda h: h.activation(out=out, in_=in_, func=func, **kw), reads, writes)

    def ACOPY(out, in_, reads, writes):
        T.op("act", lambda h: h.copy(out=out, in_=in_), reads, writes)

    def TT(eng, out, a, b, op, reads, writes):
        T.op(eng, lambda h: h.tensor_tensor(out=out, in0=a, in1=b, op=op), reads, writes)

    def TS(eng, out, a, s1, s2, op0, op1, reads, writes):
        if op1 is None:
            T.op(eng, lambda h: h.tensor_scalar(out=out, in0=a, scalar1=s1, scalar2=None, op0=op0), reads, writes)
        else:
            T.op(eng, lambda h: h.tensor_scalar(out=out, in0=a, scalar1=s1, scalar2=s2, op0=op0, op1=op1), reads, writes)

    def STT(out, a, s, b, op0, op1, reads, writes):
        T.op("dve", lambda h: h.scalar_tensor_tensor(out=out, in0=a, scalar=s, in1=b, op0=op0, op1=op1), reads, writes)

    def RECIP(out, in_, reads, writes):
        T.op("dve", lambda h: h.reciprocal(out=out, in_=in_), reads, writes)

    def MM(ps, lhsT, rhs, start, stop, reads, writes, inc=False):
        T.op("pe", lambda h: h.matmul(ps, lhsT=lhsT, rhs=rhs, start=start, stop=stop), reads, writes,
             inc=(stop or inc))

    def DMA(out, in_, dst, reads=()):
        T.dma([lambda h: h.dma_start(out=out, in_=in_)], dst, reads)

    class WS:
        def __init__(self):
            self.stb = [Buf("st%d" % i) for i in range(NS_)]
            self.bfb = [Buf("bf%d" % i) for i in range(NB_)]
            self.rec = []; self.i = 0; self.issued = 0; self.fences = []; self.pending = []

        def fence(self):
            self.rec.append("FENCE")
            self.i += 1
            if plan is not None:
                self.issued = max(self.issued, self.i)

        def _src(self, seg):
            name, idx, k0, nk, c0, ncols = seg
            W = WD[name][idx]
            return W[k0 * 128:(k0 + nk) * 128, c0:c0 + ncols].rearrange("(k p) c -> p k c", p=128)

        def _issue(self, t, now=False):
            tile = plan[t]; s = t % NS_; b = t % NB_; off = 0; fns = []
            for seg in tile:
                nk, ncols = seg[3], seg[5]
                dst = wst[s][:, off:off + nk * ncols].rearrange("p (k c) -> p k c", k=nk)
                src = self._src(seg)
                fns.append(lambda h, dst=dst, src=src: h.dma_start(out=dst, in_=src))
                off += nk * ncols
            T.dma(fns, self.stb[s])
            o_ = wbf[b][:, 0:off]; i_ = wst[s][:, 0:off]
            eng = ("act", "dve")[t % 2]

            def cast(eng=eng, o_=o_, i_=i_, s=s, b=b):
                if eng == "act":
                    T.op("act", lambda h: h.copy(out=o_, in_=i_), [self.stb[s]], [self.bfb[b]])
                else:
                    T.op(eng, lambda h: h.tensor_copy(out=o_, in_=i_), [self.stb[s]], [self.bfb[b]])
            if eng == "pool" or now:
                cast()
            else:
                self.pending.append(cast)

        def next(self, tile):
            t = self.i; self.i += 1
            self.rec.append(tile)
            if plan is not None:
                assert plan[t] == tile, (t, plan[t], tile)
                for c_ in self.pending:
                    c_()
                self.pending = []
                while self.issued <= min(t + LA_, len(plan) - 1):
                    if plan[self.issued] == "FENCE":
                        if self.issued > t:
                            break
                        self.issued += 1
                        continue
                    self._issue(self.issued, now=(self.issued == t)); self.issued += 1
            b = t % NB_; off = 0; views = []
            for seg in tile:
                nk, ncols = seg[3], seg[5]
                views.append(wbf[b][:, off:off + nk * ncols].rearrange("p (k c) -> p k c", k=nk))
                off += nk * ncols
            return views, self.bfb[b]

    ws = WS()

    def run_tile(segs, units, tcs, evac, wfn=None):
        views, wb = ws.next(segs)
        groups = []
        for ui, unit in enumerate(units):
            for tc in tcs:
                for bi, (si, lc, rhs_fn) in enumerate(unit):
                    ps_ap, ps_b = next_ps()
                    if wfn is not None:
                        ps_ap = ps_ap[:, 0:wfn(tc)]
                    groups.append((ui, tc, si, lc, rhs_fn, ps_ap, ps_b, bi == len(unit) - 1))
        banks = []
        for gi, (ui, tc, si, lc, rhs_fn, ps_ap, ps_b, last) in enumerate(groups):
            nk = segs[si][3]
            extra = [groups[gi + 1][6]] if (gi % 2 == 0 and gi + 1 < len(groups)) else []
            for k in range(nk):
                r_ap, r_b = rhs_fn(k, tc)
                MM(ps_ap, views[si][:, k, lc:lc + 128], r_ap, k == 0, k == nk - 1, [wb, r_b],
                   [ps_b] + (extra if k == 0 else []))
            banks.append((ps_ap, ps_b))
            if last:
                evac(ui, tc, banks)
                banks = []

    DMA(prm[:, :], prm_d, prmb)
    DMA(ident[:, :], ident_d, constb)
    for tc in range(NTC):
        DMA(x[:, :, tc * TCW:(tc + 1) * TCW],
            xT_d[:, tc * TCW:(tc + 1) * TCW].rearrange("(j p) t -> p j t", p=128), xb[tc])
    T.op("dve", lambda h: h.memset(ones_bf[:, :], 1.0), [], [constb])
    T.op("dve", lambda h: h.memset(ones_f[:, :], 1.0), [], [constb])
    for r in range(2):
        for tc in range(NTC):
            i = (r * NTC + tc) % 4
            DMA(tmpf[i], rope_d[r][:, tc * TCW:(tc + 1) * TCW], tmpfb[i])
            ACOPY(ropeT[:, r, tc * TCW:(tc + 1) * TCW], tmpf[i], [tmpfb[i]], [ropeb])
    epsc = prm[:, O_EPS:O_EPS + 1]

    def stats(tc):
        ps_ap, ps_b = next_ps()
        for j in range(8):
            sq, sqb = next_sq()
            ACT(sq, x[:, j, tc * TCW:(tc + 1) * TCW], AF.Square, [xb[tc]], [sqb])
            MM(ps_ap, ones_bf[:, :], sq, j == 0, j == 7, [sqb, constb], [ps_b], inc=True)
        rs = rstd[:, tc * TCW:(tc + 1) * TCW]
        ACT(rs, ps_ap, AF.Sqrt, [ps_b, prmb], [rstdb[tc]], scale=1.0 / D, bias=epsc)
        RECIP(rs, rs, [rstdb[tc]], [rstdb[tc]])

    def apply_norm(gcol, tc, out_fn):
        for j in range(8):
            o_ap, o_b = out_fn(j)
            STT(o_ap, x[:, j, tc * TCW:(tc + 1) * TCW], prm[:, gcol + j:gcol + j + 1],
                rstd[:, tc * TCW:(tc + 1) * TCW], ALU.mult, ALU.mult, [xb[tc], rstdb[tc], prmb], [o_b])

    hT = view(R0, BF16, [8, S]); hTb = [Buf("h%d" % i) for i in range(NTC)]

    def full_norm(gcol):
        for tc in range(NTC):
            stats(tc)
        for tc in range(NTC):
            apply_norm(gcol, tc, lambda j, tc=tc: (hT[:, j, tc * TCW:(tc + 1) * TCW], hTb[tc]))

    def rh_hT(k, tc):
        return hT[:, k, tc * TCW:(tc + 1) * TCW], hTb[tc]

    def x_add_evac(jfn):
        def ev(ui, tc, banks):
            j = jfn(ui)
            xs = x[:, j, tc * TCW:(tc + 1) * TCW]
            TT("dve", xs, xs, banks[0][0], ALU.add, [banks[0][1], xb[tc]], [xb[tc]])
        return ev

    def dump_x():
        for tc in range(NTC):
            DMA(out_d[:, tc * TCW:(tc + 1) * TCW].rearrange("(j p) t -> p j t", p=128),
                x[:, :, tc * TCW:(tc + 1) * TCW], outb, [xb[tc]])

    outb = Buf("out")
    stopped = [False]

    def check_stop(name):
        if debug_stop == name and not stopped[0]:
            stopped[0] = True
            return True
        return False

    def groups_of(n, g):
        out = []; a = 0
        while a < n:
            out.append((a, min(g, n - a))); a += g
        return out

    for l in range(DEPTH):
        if stopped[0]:
            break
        P0 = l * PL
        lam_init = 0.8 - 0.6 * math.exp(-0.3 * l)
        TS("dve", scr[:, 0:1], prm[:, P0 + O_GSUB:P0 + O_GSUB + 1], 1.0 - lam_init, None, ALU.mult, None,
           [prmb], [scrb])
        lamv = prm[:, P0 + O_LAM:P0 + O_LAM + 256]
        TT("dve", tmpf[0][:, 0:64], lamv[:, 0:64], lamv[:, 64:128], ALU.mult, [prmb], [tmpfb[0]])
        TT("dve", tmpf[0][:, 64:128], lamv[:, 128:192], lamv[:, 192:256], ALU.mult, [prmb, tmpfb[0]], [tmpfb[0]])
        T.op("dve", lambda h: h.reduce_sum(out=scr[:, 2:3], in_=tmpf[0][:, 0:64], axis=mybir.AxisListType.X),
             [tmpfb[0], scrb], [scrb])
        T.op("dve", lambda h: h.reduce_sum(out=scr[:, 3:4], in_=tmpf[0][:, 64:128], axis=mybir.AxisListType.X),
             [tmpfb[0], scrb], [scrb])
        ACT(scr[:, 4:6], scr[:, 2:4], AF.Exp, [scrb], [scrb])
        TT("dve", scr[:, 6:7], scr[:, 5:6], scr[:, 4:5], ALU.subtract, [scrb], [scrb])
        TS("dve", scr[:, 1:2], scr[:, 6:7], -lam_init, None, ALU.add, None, [scrb], [scrb])
        gsubp = scr[:, 0:1]; neglam = scr[:, 1:2]

        full_norm(P0 + O_GMIX)
        y = view(R1, F32, [4, S]); yb = [Buf("y%d" % j) for j in range(4)]
        zpad = [view(R2 + i * 4160, BF16, [2080]) for i in range(2)]; zb = [Buf("z0"), Buf("z1")]
        dgm = view(R2 + 8320, BF16, [CK, 128]); dgb = Buf("diag")
        cT = view(R2, BF16, [4, S]); cb = [Buf("c%d" % i) for i in range(NTC)]
        for j in range(4):
            zi = j % 2
            if j < 2:
                T.op("dve", lambda h, zi=zi: h.memset(zpad[zi][:, 0:15], 0.0), [], [zb[zi]])
                T.op("dve", lambda h, zi=zi: h.memset(zpad[zi][:, 15 + S:2080], 0.0), [zb[zi]], [zb[zi]])
            cw0 = P0 + O_CW + j * CK
            for k in range(CK):
                TS("dve", dgm[:, k, :], ident[:, :], prm[:, cw0 + k:cw0 + k + 1], None, ALU.mult, None,
                   [constb, prmb], [dgb])

            def ev_z(ui, tc, banks, zi=zi):
                ACT(tmpf[tc % 2], banks[1][0], AF.Sigmoid, [banks[1][1]], [tmpfb[tc % 2]])
                TT("dve", zpad[zi][:, 15 + tc * TCW:15 + (tc + 1) * TCW], banks[0][0], tmpf[tc % 2], ALU.mult,
                   [banks[0][1], tmpfb[tc % 2], zb[zi]], [zb[zi]])
            run_tile([("w_in", l, 0, 8, 1536 + j * 128, 128), ("w_in", l, 0, 8, 2048 + j * 128, 128)],
                     [[(0, 0, rh_hT), (1, 0, rh_hT)]], range(NTC), ev_z)
            for tc in range(NTC):
                ps_ap, ps_b = next_ps()
                for k in range(CK):
                    MM(ps_ap, dgm[:, k, :], zpad[zi][:, tc * TCW + k:tc * TCW + k + TCW], k == 0, k == CK - 1,
                       [dgb, zb[zi]], [ps_b])
                TS("dve", y[:, j, tc * TCW:(tc + 1) * TCW], ps_ap, prm[:, P0 + O_CB + j:P0 + O_CB + j + 1], None,
                   ALU.add, None, [ps_b, prmb], [yb[j]])
        T.barrier()
        for tc in range(NTC):
            sl = slice(tc * TCW, (tc + 1) * TCW)
            pm, pmb = next_ps(); pq, pqb = next_ps()
            for j in range(4):
                a_, ab_ = next_sq()
                ACOPY(a_, y[:, j, sl], [yb[j]], [ab_])
                MM(pm, ones_bf[:, :], a_, j == 0, j == 3, [ab_, constb], [pmb], inc=True)
                s_, sb_ = next_sq()
                ACT(s_, y[:, j, sl], AF.Square, [yb[j]], [sb_])
                MM(pq, ones_bf[:, :], s_, j == 0, j == 3, [sb_, constb], [pqb], inc=True)
            mean, meanb = tmpf[0], tmpfb[0]; rs_, rsb_ = tmpf[1], tmpfb[1]
            TS("dve", mean, pm, 1.0 / CCH, None, ALU.mult, None, [pmb], [meanb])
            TT("dve", rs_, mean, mean, ALU.mult, [meanb], [rsb_])
            STT(rs_, pq, 1.0 / CCH, rs_, ALU.mult, ALU.subtract, [pqb, rsb_], [rsb_])
            ACT(rs_, rs_, AF.Sqrt, [rsb_, prmb], [rsb_], scale=1.0, bias=epsc)
            RECIP(rs_, rs_, [rsb_], [rsb_])
            for j in range(4):
                t_, tb_ = tmpf[2 + j % 2], tmpfb[2 + j % 2]
                TT("dve", t_, y[:, j, sl], mean, ALU.subtract, [yb[j], meanb], [tb_])
                TT("dve", t_, t_, rs_, ALU.mult, [tb_, rsb_], [tb_])
                ACT(cT[:, j, sl], t_, AF.Silu, [tb_, prmb], [cb[tc]],
                    scale=prm[:, P0 + O_LNG + j:P0 + O_LNG + j + 1], bias=prm[:, P0 + O_LNB + j:P0 + O_LNB + j + 1])
        T.barrier()
        kT = view(R1, BF16, [4, S]); kb = [Buf("k%d" % i) for i in range(NTC)]
        vT = view(R1 + 16384, BF16, [16, 512]); vb = [Buf("v%d" % i) for i in range(NTC)]
        for h_ in range(NH):
            def ev_k(ui, tc, banks, h_=h_):
                sl = slice(tc * TCW, (tc + 1) * TCW)
                TT("dve", tmpf[0], banks[0][0], ropeT[:, 0, sl], ALU.mult, [banks[0][1], ropeb], [tmpfb[0]])
                TT("dve", tmpf[1], banks[1][0], ropeT[:, 1, sl], ALU.mult, [banks[1][1], ropeb], [tmpfb[1]])
                TT("dve", kT[:, h_, sl], tmpf[0], tmpf[1], ALU.add, [tmpfb[0], tmpfb[1]], [kb[tc]])
            run_tile([("w_in", l, 0, 8, 512 + h_ * 128, 128), ("w_qkp", l, 0, 8, 512 + h_ * 128, 128)],
                     [[(0, 0, rh_hT), (1, 0, rh_hT)]], range(NTC), ev_k)
        for half in range(2):
            views, wb = ws.next([("w_in", l, 0, 8, 1024 + half * 256, 256)])
            vpb = [next_ps() for _ in range(16)]
            for tk in range(16):
                ps_ap, ps_b = vpb[tk]
                for k in range(8):
                    MM(ps_ap[:, 0:256], hT[:, k, tk * 128:(tk + 1) * 128], views[0][:, k, :], k == 0, k == 7,
                       [wb, hTb[tk // 4]], [ps_b] + ([vpb[tk + 1][1]] if (k == 0 and tk % 2 == 0) else []))
                ACOPY(vT[:, tk, half * 256:(half + 1) * 256], ps_ap[:, 0:256], [ps_b], [vb[tk // 4]])
        T.barrier()
        hq = view(R0, BF16, [8, TCW]); hqb = Buf("hq")
        qq = view(R0 + 8192, BF16, [4, TCW]); qqb = [Buf("q%d" % i) for i in range(NH)]
        ex = [view(R0 + 12288 + i * 1024, BF16, [TCW]) for i in range(4)]; exb = [Buf("e%d" % i) for i in range(4)]
        on = view(R0 + 16384, BF16, [4, TCW]); onb = [Buf("on%d" % i) for i in range(NH)]
        mg = view(R0 + 20480, BF16, [8, TCW]); mgb = [Buf("m%d" % i) for i in range(8)]
        itc = [0]
        for qc in range(NTC):
            qsl = slice(qc * TCW, (qc + 1) * TCW)
            if qc == 0:
                apply_norm(P0 + O_GMIX, qc, lambda j: (hq[:, j, :], hqb))

            def rh_hq(k, tc):
                return hq[:, k, :], hqb
            for h_ in range(NH):
                def ev_q(ui, tc, banks, h_=h_):
                    TT("dve", tmpf[0], banks[0][0], ropeT[:, 0, qsl], ALU.mult, [banks[0][1], ropeb], [tmpfb[0]])
                    TT("dve", tmpf[1], banks[1][0], ropeT[:, 1, qsl], ALU.mult, [banks[1][1], ropeb], [tmpfb[1]])
                    TT("dve", qq[:, h_, :], tmpf[0], tmpf[1], ALU.add, [tmpfb[0], tmpfb[1]], [qqb[h_]])
                run_tile([("w_in", l, 0, 8, h_ * 128, 128), ("w_qkp", l, 0, 8, h_ * 128, 128)],
                         [[(0, 0, rh_hq), (1, 0, rh_hq)]], [0], ev_q)
            if qc >= 1:
                stats(qc - 1)
            for h_ in range(NH):
                for c_ in range(2):
                    it = itc[0]; itc[0] += 1
                    rows = slice(c_ * 64, (c_ + 1) * 64)
                    pU, pUb = psum[3 + it % 2], psb[3 + it % 2]
                    pD, pDb = psum[5 + it % 2], psb[5 + it % 2]

                    SBK = (0, 1, 2, 7)

                    def S_(kt, extra_r=(), extra_w=()):
                        b_ = SBK[kt % 4]
                        MM(psum[b_], kT[rows, h_, kt * 128:(kt + 1) * 128], qq[rows, h_, :], True, True,
                           [kb[kt // 4], qqb[h_]] + list(extra_r), [psb[b_]] + list(extra_w))
                    S_(0, extra_w=[psb[SBK[1]]]); S_(1)
                    for p_ in range(8):
                        k0, k1 = 2 * p_, 2 * p_ + 1
                        for kt in (k0, k1):
                            ACT(ex[kt % 4], psum[SBK[kt % 4]], AF.Exp, [psb[SBK[kt % 4]]], [exb[kt % 4]],
                                scale=HD ** -0.5)
                        if k0 + 2 < 16:
                            S_(k0 + 2, extra_r=[exb[k0 % 4], exb[k1 % 4], vb[k0 // 4], vb[k1 // 4]],
                               extra_w=[psb[SBK[(k0 + 3) % 4]]])
                            S_(k0 + 3)
                        for kt in (k0, k1):
                            e_, eb_ = ex[kt % 4], exb[kt % 4]
                            MM(pU, vT[:, kt, h_ * 128:(h_ + 1) * 128], e_, kt == 0, kt == 15, [vb[kt // 4], eb_], [pUb])
                            MM(pD, ones_bf[:, :], e_, kt == 0, kt == 15, [constb, eb_], [pDb], inc=True)
                    RECIP(tmpf[2], pD, [pDb], [tmpfb[2]])
                    if c_ == 0:
                        TT("dve", tmpf[0], pU, tmpf[2], ALU.mult, [pUb, tmpfb[2]], [tmpfb[0]])
                    else:
                        TT("dve", tmpf[1], pU, tmpf[2], ALU.mult, [pUb, tmpfb[2]], [tmpfb[1]])
                        STT(tmpf[0], tmpf[1], neglam, tmpf[0], ALU.mult, ALU.add, [tmpfb[1], tmpfb[0], scrb], [tmpfb[0]])
                s_, sb_ = next_sq()
                ACT(s_, tmpf[0], AF.Square, [tmpfb[0]], [sb_])
                stk = 5 + itc[0] % 2
                MM(psum[stk], ones_bf[:, :], s_, True, True, [sb_, constb], [psb[stk]])
                ACT(tmpf[3], psum[stk], AF.Sqrt, [psb[stk], prmb], [tmpfb[3]], scale=1.0 / VD, bias=epsc)
                RECIP(tmpf[3], tmpf[3], [tmpfb[3]], [tmpfb[3]])
                STT(on[:, h_, :], tmpf[0], gsubp, tmpf[3], ALU.mult, ALU.mult, [tmpfb[0], tmpfb[3], scrb], [onb[h_]])

            def rh_on(k, tc):
                return on[:, k, :], onb[k]

            def rh_c(k, tc):
                return cT[:, k, qsl], cb[qc]
            for j in range(8):
                def ev_a(ui, tc, banks, j=j):
                    ACT(tmpf[0], banks[0][0], AF.Sigmoid, [banks[0][1]], [tmpfb[0]])
                    TT("dve", tmpf[1], tmpf[0], banks[1][0], ALU.mult, [tmpfb[0], banks[1][1]], [tmpfb[1]])
                run_tile([("w_in", l, 0, 8, 2560 + j * 128, 128), ("w_ao", l, 0, 4, j * 128, 128)],
                         [[(0, 0, rh_hq), (1, 0, rh_on)]], [0], ev_a)

                def ev_b(ui, tc, banks, j=j):
                    ACT(tmpf[2], banks[0][0], AF.Sigmoid, [banks[0][1]], [tmpfb[2]])
                    TT("dve", tmpf[3], tmpf[2], banks[1][0], ALU.mult, [tmpfb[2], banks[1][1]], [tmpfb[3]])
                    TT("dve", mg[:, j, :], tmpf[1], tmpf[3], ALU.add, [tmpfb[1], tmpfb[3]], [mgb[j]])
                run_tile([("w_in", l, 0, 8, 3584 + j * 128, 128), ("w_co", l, 0, 4, j * 128, 128)],
                         [[(0, 0, rh_hq), (1, 0, rh_c)]], [0], ev_b)

            def rh_m(k, tc):
                return mg[:, k, :], mgb[k]
            if qc + 1 < NTC:
                apply_norm(P0 + O_GMIX, qc + 1, lambda j: (hq[:, j, :], hqb))
            for ct in range(4):
                run_tile([("w_o", l, 0, 8, ct * 256, 256)], [[(0, 0, rh_m)], [(0, 128, rh_m)]], [qc],
                         x_add_evac(lambda ui, ct=ct: ct * 2 + ui))
        stats(NTC - 1)
        T.barrier()
        if check_stop("mix%d" % l):
            break
        act = view(R1, BF16, [8, S]); actb = [Buf("a%d" % i) for i in range(NTC)]

        def rh_act(k, tc):
            return act[:, k, tc * TCW:(tc + 1) * TCW], actb[tc]

        def ffn(gu_name, dn_name, widx, F, down_evac):
            for (g0, gn) in groups_of(F // 128, 8):
                for jj in range(gn):
                    j = g0 + jj

                    def ev_gu(ui, tc, banks, jj=jj):
                        s_, sb__ = tmpf[tc % 2], tmpfb[tc % 2]
                        ACT(s_, banks[0][0], AF.Silu, [banks[0][1]], [sb__])
                        TT("dve", act[:, jj, tc * TCW:(tc + 1) * TCW], s_, banks[1][0], ALU.mult,
                           [sb__, banks[1][1]], [actb[tc]])
                    run_tile([(gu_name, widx, 0, 8, j * 128, 128), (gu_name, widx, 0, 8, F + j * 128, 128)],
                             [[(0, 0, rh_hT), (1, 0, rh_hT)]], range(NTC), ev_gu)
                for ct in range(4):
                    run_tile([(dn_name, widx, g0, gn, ct * 256, 256)], [[(0, 0, rh_act)], [(0, 128, rh_act)]],
                             range(NTC), down_evac(ct))

        if l % 2 == 0:
            for tc in range(NTC):
                apply_norm(P0 + O_GFFN, tc, lambda j, tc=tc: (hT[:, j, tc * TCW:(tc + 1) * TCW], hTb[tc]))
            ffn("w_gu", "w_dn", 0, DFF, lambda ct: x_add_evac(lambda ui, ct=ct: ct * 2 + ui))
        else:
            h32 = view(R1, F32, [8, TCW]); h32b = Buf("h32")
            for tc in range(NTC):
                apply_norm(P0 + O_GFFN, tc, lambda j, tc=tc: (hT[:, j, tc * TCW:(tc + 1) * TCW], hTb[tc]))
                apply_norm(P0 + O_GFFN, tc, lambda j: (h32[:, j, :], h32b))
                for t4 in range(4):
                    tk = tc * 4 + t4
                    ps_ap, ps_b = next_ps()
                    for k in range(8):
                        MM(ps_ap[:, 0:8], h32[:, k, t4 * 128:(t4 + 1) * 128],
                           prm[:, O_WR + k * 8:O_WR + (k + 1) * 8], k == 0, k == 7, [h32b, prmb], [ps_b])
                    lg = rt[:, 0:8]; mx = rt[:, 8:16]; m1 = rt[:, 16:24]; m2 = rt[:, 24:32]
                    TT("dve", lg, ps_ap[:, 0:8], prm[:, O_BR:O_BR + 8], ALU.add, [ps_b, prmb, rtb], [rtb])
                    T.op("dve", lambda h, lg=lg, mx=mx: h.max(out=mx, in_=lg), [rtb], [rtb])
                    TS("dve", m1, lg, rt[:, 8:9], None, ALU.is_equal, None, [rtb], [rtb])
                    TS("dve", m2, lg, rt[:, 9:10], None, ALU.is_equal, None, [rtb], [rtb])
                    TT("dve", rt[:, 32:33], rt[:, 8:9], rt[:, 9:10], ALU.subtract, [rtb], [rtb])
                    ACT(rt[:, 33:34], rt[:, 32:33], AF.Sigmoid, [rtb], [rtb])
                    ACT(rt[:, 34:35], rt[:, 32:33], AF.Sigmoid, [rtb], [rtb], scale=-1.0)
                    TS("dve", comb[:, tk, :], m1, rt[:, 33:34], None, ALU.mult, None, [rtb, combb], [combb])
                    STT(comb[:, tk, :], m2, rt[:, 34:35], comb[:, tk, :], ALU.mult, ALU.add, [rtb, combb], [combb])
                    TT("dve", maskf[:, tk, :], m1, m2, ALU.add, [rtb, mkb], [mkb])
            T.op("dve", lambda h: h.tensor_copy(out=maskb[:, :, :], in_=maskf[:, :, :]), [mkb], [mkb])
            ps_ap, ps_b = next_ps()
            for tk in range(16):
                MM(ps_ap[:, 0:8], ones_bf[:, :], maskb[:, tk, :], tk == 0, tk == 15, [mkb, constb], [ps_b])
            T.op("dve", lambda h: h.tensor_reduce(out=rt[:, 35:36], in_=ps_ap[:, 0:8], axis=mybir.AxisListType.X,
                                                  op=ALU.max), [ps_b, rtb], [rtb])
            fl = rt[:, 40:60]
            TS("dve", fl[:, 0:1], rt[:, 35:36], float(os.environ.get("KTH0", CAP + 0.5)), None, ALU.is_gt, None, [rtb], [rtb])
            flv = fl[:, 1:17].rearrange("p (e c) -> p e c", c=2)
            TS("dve", flv[:, :, 0], ps_ap[:, 0:8], float(os.environ.get("KTH1", 512.5)), None, ALU.is_gt, None, [ps_b, rtb], [rtb])
            TS("dve", flv[:, :, 1], ps_ap[:, 0:8], float(os.environ.get("KTH2", 768.5)), None, ALU.is_gt, None, [ps_b, rtb], [rtb])
            TS("dve", fl[:, 17:20], fl[:, 0:3], 0.0, None, ALU.mult, None, [rtb], [rtb])
            T.op("dve", lambda h: h.memset(zlhs, 0.0), [], [rtb])
            T.op("dve", lambda h: h.tensor_copy(out=flagi[:, :], in_=fl), [rtb], [flagib])
            T.barrier()
            T.load_flag(flagi, flagib, 17)
            ws.fence()
            T.cond_begin()
            cbc = [view(R2 + i * 8192, F32, [S]) for i in range(2)]; cbcb = [Buf("cb0"), Buf("cb1")]
            for e in range(NE):
                ci = e % 2
                for tc in range(NTC):
                    ps_ap, ps_b = next_ps()
                    for t4 in range(4):
                        tk = tc * 4 + t4
                        dg, dgb = tmpf[2 + t4 % 2], tmpfb[2 + t4 % 2]
                        TS("dve", dg[:, 0:128], ident[:, :], comb[:, tk, e:e + 1], None, ALU.mult, None,
                           [constb, combb], [dgb])
                        MM(ps_ap[:, t4 * 128:(t4 + 1) * 128], ones_f[:, :], dg[:, 0:128], True, True,
                           [constb, dgb], [ps_b])
                    ACOPY(cbc[ci][:, tc * TCW:(tc + 1) * TCW], ps_ap, [ps_b], [cbcb[ci]])

                def dn_evac(ct, ci=ci):
                    def ev(ui, tc, banks):
                        j = ct * 2 + ui
                        t_, tb_ = tmpf[2 + (ui + tc) % 2], tmpfb[2 + (ui + tc) % 2]
                        TT("dve", t_, banks[0][0], cbc[ci][:, tc * TCW:(tc + 1) * TCW], ALU.mult,
                           [banks[0][1], cbcb[ci]], [tb_])
                        xs = x[:, j, tc * TCW:(tc + 1) * TCW]
                        TT("dve", xs, xs, t_, ALU.add, [tb_, xb[tc]], [xb[tc]])
                    return ev
                ffn("we_gu", "we_dn", e, DEX, dn_evac)

            ws.fence()
            T.cond_else()
            assert l == DEPTH - 1
            NSB = CAP // 128
            chunks = [(0, 512, None), (512, 256, 0), (768, 256, 1)]
            plo = Buf("P_act_lo"); phi = Buf("act_hi"); heb = Buf("hE_eo"); hTMb = Buf("hTM"); cstb = Buf("cst"); r4b = Buf("rhs4")
            sib = Buf("sinfo"); posb = Buf("pos"); hscrb = Buf("hscr")
            actS = view(0, BF16, [28, CAP]); Pm = view(0, BF16, [16, CAP])
            hTM = view(32768, BF16, [16, 1024])
            hE = view(65536, BF16, [8, CAP]); eo = view(65536, BF16, [NSB, 1024])
            tS = [view(81920 + i * 2048, F32, [512]) for i in range(2)]; tSb = [Buf("tS0"), Buf("tS1")]
            iota = view(87552, F32, [1024])
            ropeflat = ropeT[:, :, :].rearrange("p a b -> p (a b)")
            PT = [rstd[:, :].bitcast(BF16).rearrange("p (s t) -> p s t", s=NSB),
                  ropeflat.rearrange("p (s t) -> p s t", s=NSB)]
            PTb = [Buf("PT0"), Buf("PT1")]
            triu_b = view(91904, BF16, [128])
            rhs4 = view(92160, BF16, [16, 4])
            identb = view(92288, BF16, [128])
            DMA(iota, cst_d[:, 0:1024], cstb)
            DMA(tS[1][:, 0:128], cst_d[:, 1024:1152], tSb[1])
            DMA(tS[0][:, 0:32], cst_d[:, 1152:1184], tSb[0])
            ACOPY(triu_b, tS[1][:, 0:128], [tSb[1]], [cstb])
            ACOPY(identb, ident[:, :], [constb, cstb], [cstb])
            ACOPY(rhs4[:, :, 0:2], tS[0][:, 0:32].rearrange("p (t c) -> p t c", c=2), [tSb[0]], [r4b])
            for tk in range(16):
                for kq in range(2):
                    ps_ap, ps_b = next_ps()
                    pbf = ps_ap.bitcast(BF16)
                    for k4 in range(4):
                        k = kq * 4 + k4
                        o_ = pbf[:, k4 * 128:(k4 + 1) * 128]; i_ = hT[:, k, tk * 128:(tk + 1) * 128]
                        T.op("pe", lambda h, o_=o_, i_=i_: h.transpose(o_, i_, identb), [hTb[tk // 4], cstb], [ps_b])
                    ACOPY(hTM[:, tk, kq * 512:(kq + 1) * 512], pbf[:, 0:512], [ps_b], [hTMb])
            hflat = hTM.rearrange("p a b -> p (a b)")
            DMA(hscr_d, hflat, hscrb, [hTMb])
            for tk in range(16):
                ps_ap, ps_b = next_ps()
                MM(ps_ap[:, 0:8], triu_b, maskb[:, tk, :], True, tk == 0, [cstb, mkb], [ps_b])
                for t2 in range(tk):
                    MM(ps_ap[:, 0:8], ones_bf[:, :], maskb[:, t2, :], False, t2 == tk - 1, [constb, mkb], [ps_b])
                ACOPY(posf[:, tk, :], ps_ap[:, 0:8], [ps_b], [posb])
            T.barrier()

            def cond_chain(e, items):
                def rec(i):
                    if i >= len(items):
                        return
                    fi, fn = items[i]
                    if fi is None or not USE_SKIP:
                        fn(); rec(i + 1)
                    else:
                        T.skip_begin(1 + e * 2 + fi)
                        fn(); rec(i + 1)
                        T.skip_end()
                rec(0)

            for e in range(NE):
                if e > 0:
                    T.dma([lambda h: h.dma_start(out=hflat, in_=hscr_d)], hTMb, [hscrb], also=[phi])
                def prep_ops(e2):
                    ops = [lambda: ACOPY(rhs4[:, :, 2], comb[:, :, e2], [combb], [r4b]),
                           lambda: TT("dve", rhs4[:, :, 3], comb[:, :, e2], rhs4[:, :, 2], ALU.subtract,
                                      [combb, r4b], [r4b])]
                    for tk in range(16):
                        ops.append(lambda tk=tk: TS("dve", Pm[:, tk, :], iota, posf[:, tk, e2:e2 + 1],
                                                    maskf[:, tk, e2:e2 + 1], ALU.is_equal, ALU.mult,
                                                    [cstb, posb, mkb], [plo]))
                    return ops
                if e == 0:
                    for f_ in prep_ops(0):
                        f_()
                pend = prep_ops(e + 1) if e + 1 < NE else []
                its = []
                for (c0, W, fi) in chunks:
                    def g_(c0=c0, W=W):
                        pbs = [next_ps() for _ in range(8)]
                        for k in range(8):
                            ps_ap, ps_b = pbs[k]
                            for tk in range(16):
                                MM(ps_ap[:, 0:W], hTM[:, tk, k * 128:(k + 1) * 128], Pm[:, tk, c0:c0 + W],
                                   tk == 0, tk == 15, [hTMb, plo],
                                   [ps_b] + ([pbs[k + 1][1]] if (tk == 0 and k % 2 == 0) else []))
                            if k % 2 == 0:
                                ACOPY(hE[:, k, c0:c0 + W], ps_ap[:, 0:W], [ps_b], [heb])
                            else:
                                T.op("dve", lambda h, o_=hE[:, k, c0:c0 + W], i_=ps_ap[:, 0:W]:
                                     h.tensor_copy(out=o_, in_=i_), [ps_b], [heb])
                    its.append((fi, g_))
                cond_chain(e, its)
                for sb_ in range(NSB):
                    ps_ap, ps_b = next_ps()
                    for tk in range(16):
                        MM(ps_ap[:, 0:4], Pm[:, tk, sb_ * 128:(sb_ + 1) * 128], rhs4[:, tk, :], tk == 0, tk == 15,
                           [plo, r4b], [ps_b])
                    ACOPY(sinfo[:, sb_, :], ps_ap[:, 0:4], [ps_b], [sib])
                TT("dve", tokw[:, 0, :], sinfo[:, :, 0], sinfo[:, :, 1], ALU.add, [sib], [sib])
                TT("dve", tokw[:, 1, :], sinfo[:, :, 2], sinfo[:, :, 3], ALU.add, [sib], [sib])

                def build_pt(tc):
                    pt, ptb = PT[tc % 2], PTb[tc % 2]
                    TS("dve", tokw[:, 2, :], tokw[:, 0, :], float(-tc * TCW), None, ALU.add, None, [sib], [sib])
                    for sb_ in range(NSB):
                        TS("dve", pt[:, sb_, :], iota[:, 0:TCW], tokw[:, 2, sb_:sb_ + 1], None, ALU.is_equal, None,
                           [cstb, sib], [ptb])
                build_pt(0); build_pt(1)
                for j in range(DEX // 128):
                    views, wb = ws.next([("we_gu", e, 0, 8, j * 128, 128), ("we_gu", e, 0, 8, DEX + j * 128, 128)])
                    its = []
                    for ci, (c0, W, fi) in enumerate(chunks):
                        def gu_(c0=c0, W=W, ci=ci, j=j, views=views, wb=wb):
                            pg, pgb = next_ps(); pu, pub = next_ps()
                            for (si, pp_, ppb_) in ((0, pg, pgb), (1, pu, pub)):
                                for k in range(8):
                                    MM(pp_[:, 0:W], views[si][:, k, 0:128], hE[:, k, c0:c0 + W], k == 0, k == 7,
                                       [wb, heb], [ppb_] + ([pub] if (si == 0 and k == 0) else []))
                            t_, tb_ = tS[ci % 2], tSb[ci % 2]
                            ACT(t_[:, 0:W], pg[:, 0:W], AF.Silu, [pgb], [tb_])
                            TT("dve", actS[:, j, c0:c0 + W], t_[:, 0:W], pu[:, 0:W], ALU.mult, [tb_, pub], [plo, phi])
                        its.append((fi, gu_))
                    cond_chain(e, its)
                sbg = [(0, 4, None), (4, 2, 0), (6, 2, 1)]
                for dh in range(2):
                    for kg in range(7):
                        views, wb = ws.next([("we_dn", e, kg * 4, 4, dh * 512, 512)])
                        its = []
                        for (s0, sn, fi) in sbg:
                            def dn_(s0=s0, sn=sn, kg=kg, views=views, wb=wb):
                                for sb_ in range(s0, s0 + sn):
                                    for k4 in range(4):
                                        MM(psum[sb_], actS[:, kg * 4 + k4, sb_ * 128:(sb_ + 1) * 128],
                                           views[0][:, k4, :], kg == 0 and k4 == 0, kg == 6 and k4 == 3,
                                           [wb, plo, phi], [psb[sb_]], inc=(sb_ == s0 + sn - 1 and k4 == 3))
                            its.append((fi, dn_))
                        cond_chain(e, its)
                    its = []
                    for (s0, sn, fi) in sbg:
                        def de_(s0=s0, sn=sn, dh=dh):
                            for sb_ in range(s0, s0 + sn):
                                if sb_ % 2 == 0:
                                    TS("dve", eo[:, sb_, dh * 512:(dh + 1) * 512], psum[sb_], tokw[:, 1, sb_:sb_ + 1],
                                       None, ALU.mult, None, [psb[sb_], sib], [heb])
                                else:
                                    ACT(eo[:, sb_, dh * 512:(dh + 1) * 512], psum[sb_], AF.Copy, [psb[sb_], sib], [heb],
                                        scale=tokw[:, 1, sb_:sb_ + 1])
                        its.append((fi, de_))
                    cond_chain(e, its)
                for tc in range(NTC):
                    pt, ptb = PT[tc % 2], PTb[tc % 2]
                    if 1 <= tc <= 2:
                        build_pt(tc + 1)
                    spb = [next_ps() for _ in range(8)]
                    for j in range(8):
                        ps_ap, ps_b = spb[j]
                        its = []
                        for (s0, sn, fi) in sbg:
                            def sc_(s0=s0, sn=sn, j=j, ps_ap=ps_ap, ps_b=ps_b, last=(s0 + sn == NSB), st=(s0 == 0)):
                                for sb_ in range(s0, s0 + sn):
                                    MM(ps_ap, eo[:, sb_, j * 128:(j + 1) * 128], pt[:, sb_, :], st and sb_ == s0,
                                       False, [heb, ptb],
                                       [ps_b] + ([spb[j + 1][1]] if (st and sb_ == s0 and j % 2 == 0) else []),
                                       inc=(sb_ == s0 + sn - 1))
                            its.append((fi, sc_))
                        cond_chain(e, its)
                        MM(ps_ap, zlhs, pt[:, 0, :], False, True, [cstb, ptb], [ps_b])
                        xs = x[:, j, tc * TCW:(tc + 1) * TCW]
                        TT("dve", xs, xs, ps_ap, ALU.add, [ps_b, xb[tc]], [xb[tc]])
                        if pend:
                            pend.pop(0)()
                while pend:
                    pend.pop(0)()
            ws.fence()
            T.cond_end()
        T.barrier()
        if check_stop("ffn%d" % l):
            break
        full_norm(P0 + O_GPLE)
        pTb = view(R1, BF16, [2, S]); pb = [Buf("p%d" % i) for i in range(NTC)]
        pst = [view(R1 + 8192 + i * 4096, F32, [2, TCW]) for i in range(2)]; pstb = [Buf("ps0"), Buf("ps1")]
        for tc in range(NTC):
            i = tc % 2
            DMA(pst[i], pT_d[l][:, tc * TCW:(tc + 1) * TCW].rearrange("(j p) t -> p j t", p=128), pstb[i])
            ACOPY(pTb[:, :, tc * TCW:(tc + 1) * TCW], pst[i], [pstb[i]], [pb[tc]])

        def rh_p(k, tc):
            return pTb[:, k, tc * TCW:(tc + 1) * TCW], pb[tc]
        for j in range(8):
            def ev_p(ui, tc, banks, j=j):
                s_, sb__ = tmpf[tc % 2], tmpfb[tc % 2]
                t_, tb_ = tmpf[2 + tc % 2], tmpfb[2 + tc % 2]
                ACT(s_, banks[0][0], AF.Sigmoid, [banks[0][1]], [sb__])
                TT("dve", t_, s_, banks[1][0], ALU.mult, [sb__, banks[1][1]], [tb_])
                xs = x[:, j, tc * TCW:(tc + 1) * TCW]
                TT("dve", xs, xs, t_, ALU.add, [tb_, xb[tc]], [xb[tc]])
            run_tile([("w_pg", l, 0, 8, j * 128, 128), ("w_pp", l, 0, 2, j * 128, 128)],
                     [[(0, 0, rh_hT), (1, 0, rh_p)]], range(NTC), ev_p)
        T.barrier()
        if check_stop("ple%d" % l):
            break

    if not stopped[0]:
        for tc in range(NTC):
            stats(tc)
            apply_norm(O_GFIN, tc, lambda j, tc=tc: (x[:, j, tc * TCW:(tc + 1) * TCW], xb[tc]))
    dump_x()
    fin = [(outb.sem, outb.cnt)]
    T.prog["sp"].append((fin, None, None, 0))
    if plan is None:
        es.close()
        return None, ws.rec
    assert ws.i == len(plan)
    print('prog sizes', {e: len(p) for e, p in T.prog.items()}, 'nsem', len(T.semh))
    assert T.simulate(True), 'deadlock in semaphore program (if)'
    assert T.simulate(False, True), 'deadlock in semaphore program (else, chunks on)'
    assert T.simulate(False, False), 'deadlock in semaphore program (else, chunks off)'
    T.emit()
    es.close()
    return nc, ws.rec


def _prep(inputs):
    g = lambda k: np.asarray(inputs[k], dtype=np.float32)
    x = g("x"); p = g("p")
    w_in = g("w_in")
    idx = np.arange(512)
    partner = (idx // 64) * 64 + ((idx % 64) + 32) % 64
    w_qkp = np.ascontiguousarray(np.concatenate([w_in[:, :, partner], w_in[:, :, 512 + partner]], axis=2))
    prm = np.zeros((128, NP), np.float32)

    def col8(v):
        return v.reshape(-1, 128).T

    for l in range(DEPTH):
        P0 = l * PL
        prm[:, P0 + O_GMIX:P0 + O_GMIX + 8] = col8(g("g_mix")[l])
        prm[:, P0 + O_GFFN:P0 + O_GFFN + 8] = col8(g("g_ffn")[l])
        prm[:, P0 + O_GPLE:P0 + O_GPLE + 8] = col8(g("g_ple")[l])
        prm[:, P0 + O_GSUB] = g("g_subln")[l]
        prm[:, P0 + O_CB:P0 + O_CB + 4] = col8(g("conv_b")[l])
        prm[:, P0 + O_LNG:P0 + O_LNG + 4] = col8(g("conv_ln_g")[l])
        prm[:, P0 + O_LNB:P0 + O_LNB + 4] = col8(g("conv_ln_b")[l])
        cw = g("conv_w")[l]
        for j in range(4):
            prm[:, P0 + O_CW + j * CK:P0 + O_CW + (j + 1) * CK] = cw[:, j * 128:(j + 1) * 128].T
        prm[:, P0 + O_LAM:P0 + O_LAM + 256] = np.broadcast_to(g("lam")[l].reshape(1, 256), (128, 256))
    prm[:, O_GFIN:O_GFIN + 8] = col8(g("g_final"))
    prm[:, O_BR:O_BR + 8] = np.broadcast_to(g("b_router")[0].reshape(1, 8), (128, 8))
    wr = g("w_router")[0]
    prm[:, O_WR:O_WR + 64] = wr.reshape(8, 128, 8).transpose(1, 0, 2).reshape(128, 64)
    prm[:, O_EPS] = EPS
    inv_freq = (10000.0 ** (-np.arange(0, HD, 2, dtype=np.float32) / HD)).astype(np.float32)
    ang = (np.arange(S, dtype=np.float32)[:, None] * inv_freq[None, :]).astype(np.float32)
    cos = np.cos(ang).astype(np.float32); sin = np.sin(ang).astype(np.float32)
    pp = np.arange(128)
    sign = np.where((pp % 64) < 32, -1.0, 1.0).astype(np.float32)
    rope = np.stack([cos[:, pp % 32].T, sin[:, pp % 32].T * sign[:, None]]).astype(np.float32)
    shared = {
        "w_in": w_in, "w_qkp": w_qkp, "w_attn_out": g("w_attn_out"), "w_conv_out": g("w_conv_out"),
        "w_o": g("w_o"), "w_ff_gu": g("w_ff_gu"), "w_ff_down": g("w_ff_down"),
        "we_gu": g("we_gu")[0], "we_down": g("we_down")[0], "w_ple_gate": g("w_ple_gate"),
        "w_ple_proj": g("w_ple_proj"), "prm": prm, "rope": np.ascontiguousarray(rope),
        "ident": np.eye(128, dtype=np.float32),
    }
    cst = np.zeros((128, 1184), np.float32)
    cst[:, 0:1024] = np.arange(1024, dtype=np.float32)[None, :]
    cst[:, 1024:1152] = np.triu(np.ones((128, 128), np.float32), k=1)
    tokc = np.zeros((128, 16, 2), np.float32)
    tokc[:, :, 0] = np.arange(128, dtype=np.float32)[:, None]
    tokc[:, :, 1] = (np.arange(16, dtype=np.float32) * 128.0)[None, :]
    cst[:, 1152:1184] = tokc.reshape(128, 32)
    shared["cst"] = cst
    in_maps = []
    for b in range(8):
        m = dict(shared)
        m["xT"] = np.ascontiguousarray(x[b].T)
        m["pT"] = np.ascontiguousarray(p[:, b].transpose(0, 2, 1))
        in_maps.append(m)
    return in_maps


def kernel(**inputs):
    dbg = os.environ.get("KDEBUG") or None
    _, plan = build(None, dbg)
    nc, _ = build(plan, dbg)
    in_maps = _prep(inputs)
    res = run_bass_kernel_spmd(nc, in_maps, core_ids=list(range(8)))
    out = np.stack([np.asarray(r["outT"]).T for r in res.results]).astype(np.float32)
    return out
```
